# Optimizing a Trainium2 kernel written in Bass

```python
import math
import jax, jax.numpy as jnp
from jax import lax
import numpy as np

D_MODEL = 2048
BATCH = 16
SEQ = 256
DEPTH = 1
DEC_BATCH = 2
DEC_SEQ = 4096
PAST_LEN = 256

GRID_W = 64
NK_HEADS = 16
NV_HEADS = 32
DK = 128
DV = 128
QK_W = NK_HEADS * DK
V_W = NV_HEADS * DV
QKV_W = 2 * QK_W + V_W
QKV_CONV = 3
CHUNK = 64
CONV_CH = D_MODEL
GCONV = 3
IN_SIZES = (QKV_W, V_W, 2 * NV_HEADS, 2 * NV_HEADS, CONV_CH, CONV_CH, CONV_CH, D_MODEL, D_MODEL)
IN_W = sum(IN_SIZES)
N_EXPERTS = 32
TOP_K = 4
D_FF = D_MODEL
SWIGLU_LIMIT = 7.0
SWIGLU_ALPHA = 1.702
MOE_BLOCK = 128
DEEPNORM_ALPHA = (2 * DEPTH) ** 0.25
DEEPNORM_BETA = (8 * DEPTH) ** -0.25
EPS = 1e-6
POS_BASE = 10000.0

kernel_name = 'hybrid_deltanet_shortconv_moe_diffusion_step'


def layer_norm(x, g, b):
    xf = x.astype(jnp.float32)
    mu = xf.mean(-1, keepdims=True)
    var = jnp.square(xf - mu).mean(-1, keepdims=True)
    return ((xf - mu) * lax.rsqrt(var + EPS) * g.astype(jnp.float32) + b.astype(jnp.float32)).astype(x.dtype)


def l2norm(t):
    return t * lax.rsqrt(jnp.sum(t * t, -1, keepdims=True) + EPS)


def dwconv_centred(x, w):
    k = w.shape[0]
    return lax.conv_general_dilated(x, w[:, None, :].astype(x.dtype), (1,), [(k // 2, k // 2)],
                                    dimension_numbers=('NWC', 'WIO', 'NWC'), feature_group_count=x.shape[-1])


def grid_pos_embed(rows, dim):
    quarter = dim // 4
    freqs = 1.0 / (POS_BASE ** (jnp.arange(quarter, dtype=jnp.float32) / quarter))
    def axis_embed(n):
        ang = jnp.arange(n, dtype=jnp.float32)[:, None] * freqs[None, :]
        return jnp.concatenate([jnp.sin(ang), jnp.cos(ang)], axis=-1)
    er = axis_embed(rows)
    ec = axis_embed(GRID_W)
    pos = jnp.concatenate([jnp.broadcast_to(er[:, None, :], (rows, GRID_W, dim // 2)),
                           jnp.broadcast_to(ec[None, :, :], (rows, GRID_W, dim // 2))], axis=-1)
    return pos.reshape(rows * GRID_W, dim)


def gated_delta_chunked(q, k, v, g, beta, s0):
    bsz, t, h, _ = q.shape
    n = t // CHUNK
    def chunks(a):
        return a.reshape((bsz, n, CHUNK, h) + a.shape[3:]).transpose((1, 0, 3, 2) + tuple(range(4, a.ndim + 1)))
    qc = chunks(q * DK ** -0.5)
    kc = chunks(k)
    vc = chunks(v)
    gc = jnp.cumsum(chunks(g), axis=-1)
    bc = chunks(beta)
    causal = jnp.tril(jnp.ones((CHUNK, CHUNK), bool))
    strict = jnp.tril(jnp.ones((CHUNK, CHUNK), bool), -1)
    decay = jnp.exp(jnp.where(causal, gc[..., :, None] - gc[..., None, :], -jnp.inf))
    kb = kc * bc[..., None]
    lmat = jnp.where(strict, jnp.einsum('nbhid,nbhjd->nbhij', kb, kc) * decay, 0.0)
    rhs = jnp.concatenate([vc * bc[..., None], kb * jnp.exp(gc)[..., None]], axis=-1)
    sol = lax.linalg.triangular_solve(lmat, rhs, left_side=True, lower=True, unit_diagonal=True)
    u, w = sol[..., :DV], sol[..., DV:]
    qk = jnp.where(causal, jnp.einsum('nbhid,nbhjd->nbhij', qc, kc) * decay, 0.0)
    def step(s, inp):
        q_i, k_i, u_i, w_i, g_i, a_i = inp
        v_new = u_i - jnp.einsum('bhck,bhkv->bhcv', w_i, s)
        o = (jnp.einsum('bhck,bhkv->bhcv', q_i * jnp.exp(g_i)[..., None], s)
             + jnp.einsum('bhij,bhjv->bhiv', a_i, v_new))
        g_last = g_i[..., -1:]
        s = (s * jnp.exp(g_last)[..., None]
             + jnp.einsum('bhck,bhcv->bhkv', k_i * jnp.exp(g_last - g_i)[..., None], v_new))
        return s, o
    s, o = lax.scan(step, s0, (qc, kc, u, w, gc, qk))
    return o.transpose(1, 0, 3, 2, 4).reshape(bsz, t, h, -1), s


def deltanet_branch(qkv, z, b_fb, a_fb, conv_w, a_log, dt_bias, norm_w, w_out, s_f, s_b):
    bsz, t, _ = qkv.shape
    dt = qkv.dtype
    f32 = jnp.float32
    qkv = jax.nn.silu(dwconv_centred(qkv, conv_w)).astype(f32)
    rep = NV_HEADS // NK_HEADS
    q = jnp.repeat(l2norm(qkv[..., :QK_W].reshape(bsz, t, NK_HEADS, DK)), rep, axis=2)
    k = jnp.repeat(l2norm(qkv[..., QK_W:2 * QK_W].reshape(bsz, t, NK_HEADS, DK)), rep, axis=2)
    v = qkv[..., 2 * QK_W:].reshape(bsz, t, NV_HEADS, DV)
    beta = jax.nn.sigmoid(b_fb.astype(f32)).reshape(bsz, t, 2, NV_HEADS)
    g = -jnp.exp(a_log.astype(f32)) * jax.nn.softplus(a_fb.astype(f32).reshape(bsz, t, 2, NV_HEADS) + dt_bias.astype(f32))
    o_f, s_f = gated_delta_chunked(q, k, v, g[:, :, 0], beta[:, :, 0], s_f.astype(f32))
    o_b, s_b = gated_delta_chunked(jnp.flip(q, 1), jnp.flip(k, 1), jnp.flip(v, 1),
                                   jnp.flip(g[:, :, 1], 1), jnp.flip(beta[:, :, 1], 1), s_b.astype(f32))
    o = o_f + jnp.flip(o_b, 1)
    o = (o * lax.rsqrt(jnp.mean(o * o, -1, keepdims=True) + EPS) * norm_w.astype(f32)
         * jax.nn.silu(z.astype(f32).reshape(bsz, t, NV_HEADS, DV)))
    return o.reshape(bsz, t, V_W).astype(dt) @ w_out, s_f, s_b


def routed_experts(h, router_w, router_b, w_gu, b_gu, w_down, b_down):
    n_tok, d = h.shape
    n_assign = n_tok * TOP_K
    logits = (h @ router_w).astype(jnp.float32) + router_b.astype(jnp.float32)
    top_logit, top_e = lax.top_k(logits, TOP_K)
    top_p = jax.nn.softmax(top_logit, axis=-1)
    flat_e = top_e.reshape(-1)
    order = jnp.argsort(flat_e)
    sorted_e = flat_e[order]
    counts = jnp.bincount(flat_e, length=N_EXPERTS)
    padded = (counts + MOE_BLOCK - 1) // MOE_BLOCK * MOE_BLOCK
    pad_end = jnp.cumsum(padded)
    pad_start = pad_end - padded
    grp_start = jnp.cumsum(counts) - counts
    slot = pad_start[sorted_e] + jnp.arange(n_assign) - grp_start[sorted_e]
    n_blocks = -(-(n_assign + N_EXPERTS * (MOE_BLOCK - 1)) // MOE_BLOCK)
    n_slots = n_blocks * MOE_BLOCK
    slot_tok = jnp.full((n_slots,), n_tok, jnp.int32).at[slot].set((order // TOP_K).astype(jnp.int32))
    slot_p = jnp.zeros((n_slots,), jnp.float32).at[slot].set(top_p.reshape(-1)[order])
    block_e = jnp.minimum(jnp.searchsorted(pad_end, jnp.arange(n_blocks) * MOE_BLOCK, side='right'), N_EXPERTS - 1)
    h_pad = jnp.concatenate([h, jnp.zeros((1, d), h.dtype)], axis=0)
    xb = h_pad[slot_tok].reshape(n_blocks, MOE_BLOCK, d)
    def expert_block(args):
        xblk, e = args
        gu = xblk @ w_gu[e] + b_gu[e]
        gate = jnp.minimum(gu[:, :D_FF], SWIGLU_LIMIT)
        up = jnp.clip(gu[:, D_FF:], -SWIGLU_LIMIT, SWIGLU_LIMIT)
        act = (up + 1.0) * gate * jax.nn.sigmoid(SWIGLU_ALPHA * gate)
        return act @ w_down[e] + b_down[e]
    yb = lax.map(expert_block, (xb, block_e))
    y = yb.reshape(n_slots, d) * slot_p[:, None].astype(h.dtype)
    return jax.ops.segment_sum(y, slot_tok, num_segments=n_tok + 1)[:n_tok]


def trunk_layer(x, mod, s_f, s_b, w_in, conv_qkv, a_log, dt_bias, norm_o, w_a_out, conv_b, w_b_out, w_o,
                ln1_g, ln1_b, router_w, router_b, w_gu, b_gu, w_down, b_down, ln2_g, ln2_b):
    shift1, scale1, gate1, shift2, scale2, gate2 = jnp.split(mod, 6, axis=-1)
    h = x * (1.0 + scale1) + shift1
    proj = h @ w_in
    offs = tuple(int(o) for o in np.cumsum(IN_SIZES)[:-1])
    qkv, z, b_fb, a_fb, u_b, u_c, u_x, r_a, r_b = jnp.split(proj, offs, axis=-1)
    y_a, s_f, s_b = deltanet_branch(qkv, z, b_fb, a_fb, conv_qkv, a_log, dt_bias, norm_o, w_a_out, s_f, s_b)
    y_b = (u_b * dwconv_centred(u_c * u_x, conv_b)) @ w_b_out
    mixed = (jax.nn.sigmoid(r_a) * y_a + jax.nn.sigmoid(r_b) * y_b) @ w_o
    x = layer_norm(DEEPNORM_ALPHA * x + gate1 * mixed, ln1_g, ln1_b)
    bsz, t, d = x.shape
    h2 = x * (1.0 + scale2) + shift2
    ff = routed_experts(h2.reshape(bsz * t, d), router_w, router_b, w_gu, b_gu, w_down, b_down).reshape(bsz, t, d)
    x = layer_norm(DEEPNORM_ALPHA * x + gate2 * ff, ln2_g, ln2_b)
    return x, s_f, s_b


def setup_inputs(seed: int = 0) -> dict:
    key = jax.random.key(seed)
    ks = jax.random.split(key, 32)
    f32 = jnp.float32
    def nrm(k, shape, s):
        return jax.random.normal(k, shape, f32) * s
    dt = jnp.exp(jax.random.uniform(ks[11], (DEPTH, 2, NV_HEADS), f32, math.log(1e-3), math.log(1e-1)))
    return {
        'x_prompt': nrm(ks[0], (BATCH, SEQ, D_MODEL), 1.0),
        'x_sample': nrm(ks[1], (DEC_BATCH, DEC_SEQ, D_MODEL), 1.0),
        'c': nrm(ks[2], (DEC_BATCH, D_MODEL), 1.0),
        'state_fwd': nrm(ks[3], (DEC_BATCH, DEPTH, NV_HEADS, DK, DV), 0.3),
        'state_bwd': nrm(ks[4], (DEC_BATCH, DEPTH, NV_HEADS, DK, DV), 0.3),
        'c_ctx': nrm(ks[5], (D_MODEL,), 1.0),
        'w_mod': nrm(ks[6], (DEPTH, D_MODEL, 6 * D_MODEL), D_MODEL ** -0.5),
        'b_mod': nrm(ks[7], (DEPTH, 6 * D_MODEL), 0.02),
        'w_in': nrm(ks[8], (DEPTH, D_MODEL, IN_W), D_MODEL ** -0.5),
        'conv_qkv': nrm(ks[9], (DEPTH, QKV_CONV, QKV_W), QKV_CONV ** -0.5),
        'a_log': jnp.log(jax.random.uniform(ks[10], (DEPTH, 2, NV_HEADS), f32, 1.0, 16.0)),
        'dt_bias': dt + jnp.log(-jnp.expm1(-dt)),
        'norm_o': 1.0 + nrm(ks[12], (DEPTH, DV), 0.02),
        'w_a_out': nrm(ks[13], (DEPTH, V_W, D_MODEL), V_W ** -0.5),
        'conv_b': nrm(ks[14], (DEPTH, GCONV, CONV_CH), GCONV ** -0.5),
        'w_b_out': nrm(ks[15], (DEPTH, CONV_CH, D_MODEL), CONV_CH ** -0.5),
        'w_o': nrm(ks[16], (DEPTH, D_MODEL, D_MODEL), DEEPNORM_BETA * D_MODEL ** -0.5),
        'ln1_g': 1.0 + nrm(ks[17], (DEPTH, D_MODEL), 0.02),
        'ln1_b': nrm(ks[18], (DEPTH, D_MODEL), 0.02),
        'router_w': nrm(ks[19], (DEPTH, D_MODEL, N_EXPERTS), D_MODEL ** -0.5),
        'router_b': nrm(ks[20], (DEPTH, N_EXPERTS), 0.01),
        'w_gu': nrm(ks[21], (DEPTH, N_EXPERTS, D_MODEL, 2 * D_FF), D_MODEL ** -0.5),
        'b_gu': nrm(ks[22], (DEPTH, N_EXPERTS, 2 * D_FF), 0.02),
        'w_down': nrm(ks[23], (DEPTH, N_EXPERTS, D_FF, D_MODEL), DEEPNORM_BETA * D_FF ** -0.5),
        'b_down': nrm(ks[24], (DEPTH, N_EXPERTS, D_MODEL), 0.02),
        'ln2_g': 1.0 + nrm(ks[25], (DEPTH, D_MODEL), 0.02),
        'ln2_b': nrm(ks[26], (DEPTH, D_MODEL), 0.02),
    }


def reference(x_prompt, x_sample, c, state_fwd, state_bwd, c_ctx, w_mod, b_mod, w_in, conv_qkv, a_log, dt_bias,
              norm_o, w_a_out, conv_b, w_b_out, w_o, ln1_g, ln1_b, router_w, router_b, w_gu, b_gu, w_down, b_down,
              ln2_g, ln2_b):
    zero_state = jnp.zeros((x_prompt.shape[0], NV_HEADS, DK, DV), jnp.float32)
    rows = x_sample.shape[1] // GRID_W
    xp = x_prompt
    xs = x_sample + grid_pos_embed(rows, D_MODEL).astype(x_sample.dtype)[None]
    new_f = []
    new_b = []
    for l in range(DEPTH):
        mod_ctx = (jax.nn.silu(c_ctx) @ w_mod[l] + b_mod[l])[None, None, :]
        mod_lat = (jax.nn.silu(c) @ w_mod[l] + b_mod[l])[:, None, :]
        lw = (w_in[l], conv_qkv[l], a_log[l], dt_bias[l], norm_o[l], w_a_out[l], conv_b[l], w_b_out[l], w_o[l],
              ln1_g[l], ln1_b[l], router_w[l], router_b[l], w_gu[l], b_gu[l], w_down[l], b_down[l], ln2_g[l], ln2_b[l])
        xp, sf, sb = trunk_layer(xp, mod_ctx, zero_state, zero_state, *lw)
        new_f.append(sf.astype(x_prompt.dtype))
        new_b.append(sb.astype(x_prompt.dtype))
        xs, _, _ = trunk_layer(xs, mod_lat, state_fwd[:, l], state_bwd[:, l], *lw)
    new_state_fwd = jnp.stack(new_f, axis=1)
    new_state_bwd = jnp.stack(new_b, axis=1)
    return (xp, xs, new_state_fwd, new_state_bwd)
```

```python
import numpy as np
import ml_dtypes
from contextlib import ExitStack
import concourse.bass as bass
import concourse.mybir as mybir
from concourse.bass_utils import run_bass_kernel_spmd

F32 = mybir.dt.float32
BF16 = mybir.dt.bfloat16
I32 = mybir.dt.int32
AF = mybir.ActivationFunctionType
ALU = mybir.AluOpType
AX = mybir.AxisListType
NDS = 24
NCORES = 8

D = 2048
KT = 16
NTOK = 12288
NPROMPT = 4096
NSEG = 48
NCHUNK = 192
EPS = 1e-6
ALPHA = 2.0 ** 0.25
NE = 32
WA_COLS = 1664


class Buf:
    __slots__ = ("name", "w", "r")

    def __init__(self, name=""):
        self.name = name
        self.w = None
        self.r = {}


class TL:
    def __init__(self, t, name="", psum=False):
        self.t = t
        self.b = Buf(name)
        self.psum = psum

    def __getitem__(self, idx):
        return self.t[idx]


class KB:
    def __init__(self, nc, stack):
        self.nc = nc
        self.stack = stack
        self.engs = {"pe": nc.tensor, "act": nc.scalar, "dve": nc.vector, "pool": nc.gpsimd, "sp": nc.sync}
        self.prog = {k: [] for k in self.engs}
        self.esem = {k: stack.enter_context(nc.semaphore("es_" + k)) for k in self.engs}
        self.ecnt = {k: 0 for k in self.engs}
        self.dsems = [stack.enter_context(nc.semaphore("ds_%d" % i)) for i in range(NDS)]
        self.dcnt = [0] * NDS
        self.dlast = [None] * NDS
        self.dnext = 0
        self.ntile = 0
        self.waited = {}
        self.ninst = 0

    def sb(self, shape, dtype, stack=None, name=None):
        self.ntile += 1
        name = name or ("t%d" % self.ntile)
        st = stack if stack is not None else self.stack
        return TL(st.enter_context(self.nc.sbuf_tensor(name, list(shape), dtype)), name)

    def ps(self, shape, dtype, stack=None, name=None):
        self.ntile += 1
        name = name or ("p%d" % self.ntile)
        st = stack if stack is not None else self.stack
        return TL(st.enter_context(self.nc.psum_tensor(name, list(shape), dtype)), name, psum=True)

    limit = None

    def _emit(self, eng, fn, reads, writes, dma=False):
        if self.limit is not None and self.ninst >= self.limit:
            return None
        waits = {}

        def addw(tok):
            if tok is None:
                return
            sem, val = tok
            k = id(sem)
            if k not in waits or waits[k][1] < val:
                waits[k] = (sem, val)

        for b in reads:
            ps_ = isinstance(b, TL) and b.psum
            b = b.b if isinstance(b, TL) else b
            addw(b.w)
            if ps_:
                for t in b.r.values():
                    addw(t)
        for b in writes:
            b = b.b if isinstance(b, TL) else b
            addw(b.w)
            for t in b.r.values():
                addw(t)
        if dma:
            k = self.dnext
            self.dnext = (k + 1) % NDS
            addw(self.dlast[k])
            self.dcnt[k] += 16
            tok = (self.dsems[k], self.dcnt[k])
            self.dlast[k] = tok
            incv = 16
        else:
            self.ecnt[eng] += 1
            tok = (self.esem[eng], self.ecnt[eng])
            incv = 1
        if eng == "pe":
            waits.pop(id(self.esem["pe"]), None)
        self.prog[eng].append((list(waits.values()), fn, tok, incv))
        self.ninst += 1
        for b in reads:
            b = b.b if isinstance(b, TL) else b
            k = id(tok[0])
            if k not in b.r or b.r[k][1] < tok[1]:
                b.r[k] = tok
        for b in writes:
            b = b.b if isinstance(b, TL) else b
            b.w = tok
            b.r = {}
        return tok

    def op(self, eng, fn, reads=(), writes=()):
        return self._emit(eng, fn, reads, writes)

    def dma(self, queue, out, in_, reads=(), writes=(), **kw):
        return self._emit(queue, lambda e: e.dma_start(out=out, in_=in_, **kw), reads, writes, dma=True)

    def barrier(self):
        toks = []
        for k in self.engs:
            if self.ecnt[k] > 0:
                toks.append((self.esem[k], self.ecnt[k]))
        for t in self.dlast:
            if t is not None:
                toks.append(t)
        for k in self.engs:
            self.prog[k].append((list(toks), None, None, 0))

    def replay(self, block):
        def run(engname):
            def body(e):
                waited = self.waited.setdefault(engname, {})
                for waits, fn, tok, incv in self.prog[engname]:
                    for sem, val in waits:
                        k = id(sem)
                        if waited.get(k, 0) >= val:
                            continue
                        e.wait_ge(sem, val)
                        waited[k] = val
                    if fn is None:
                        continue
                    ins = fn(e)
                    ins.then_inc(tok[0], incv)
            return body

        block.tensor(run("pe"))
        block.scalar(run("act"))
        block.vector(run("dve"))
        block.gpsimd(run("pool"))
        block.sync(run("sp"))
        for k in self.prog:
            self.prog[k] = []


def bcast_row(handle, row_off, n, parts=128):
    return bass.AP(tensor=handle, offset=row_off, ap=[[0, parts], [1, n]])


def seg_info(s):
    if s < 16:
        return 0, True, True, False, 0
    if s < 32:
        return 1, s == 16, s == 31, True, (s - 16) * 256
    return 2, s == 32, s == 47, True, (s - 32) * 256


def build(upto="all", debug=False, moe_experts=NE, a2_only=False, a2_limit=None, a2_stop=None, b_only=False, b_groups=(0, 1, 2)):
    nc = bass.Bass("TRN2", target_bir_lowering=False)
    dbg = {}
    phases = ["a0", "a1", "a2", "a3", "ag", "b"]
    last = phases.index(upto) if upto != "all" else len(phases) - 1

    need = [0]

    def din(name, shape, dt=F32):
        if last < need[0]:
            return None
        dbg.setdefault("_inputs", []).append(name)
        return nc.dram_tensor(name, list(shape), dt, kind="ExternalInput")

    def dout(name, shape, dt=F32):
        return nc.dram_tensor(name, list(shape), dt, kind="ExternalOutput")

    def dscr(name, shape, dt=F32):
        if debug and (debug is True or name in debug):
            dbg[name] = True
            return nc.dram_tensor(name, list(shape), dt, kind="ExternalOutput")
        return nc.dram_tensor(name, list(shape), dt)

    xb_in = din("xb", [1536, D])
    posb = din("posb", [512, D])
    cT = din("cT", [128, 48])
    w_modc = din("w_modc", [D, 1536])
    b_modc = din("b_modc", [3, 1536])
    consts = din("consts", [128, 3072])
    need[0] = 1
    wA = din("wA", [D, WA_COLS])
    convA = din("convA", [128, 24])
    dtb = din("dtb", [128, 8])
    alog = din("alog", [128, 8])
    need[0] = 2
    st0 = din("st0", [2, 2, 4, 128, 128])
    need[0] = 3
    normw = din("normw", [128, 512])
    need[0] = 5
    xh = din("xh", [4, D])
    posh = din("posh", [4, D])
    hmask = din("hmask", [128, 4])
    wB_s = din("wB_s", [256, 5 * D])
    convB = din("convB", [128, 48])
    wa_s = din("wa_s", [512, D])
    wb_s = din("wb_s", [256, D])
    wo_s = din("wo_s", [256, D])
    lnp = din("lnp", [4, 128, D])
    router_w = din("router_w", [D, NE])
    router_b = din("router_b", [128, NE])
    wgu_s = din("wgu_s", [4, D, 2 * D])
    bgu = din("bgu", [128, NE * 32])
    wdn_s = din("wdn_s", [4, D, D])
    b_down = din("b_down", [NE, D])
    idxg = din("idxg", [128, 96], I32)
    y_out = dout("y_out", [1536, D])
    ns_out = dout("ns_out", [2, 16, 4, 128, 128])
    BIN = ("modG", "agout", "wB", "w_a_out", "w_b_out", "w_o", "wguG00", "wguG01", "wdnG0")

    def dint(name, shape, dt=F32):
        if b_only and name in BIN:
            dbg.setdefault("_inputs", []).append(name)
            return nc.dram_tensor(name, list(shape), dt, kind="ExternalInput")
        return nc.dram_tensor(name, list(shape), dt)

    xin = dint("xin", [1536, D])
    xG = dint("xG", [NTOK, D])
    pin = dint("pin", [512, D])
    pG = dint("pG", [4096, D])
    modin = dint("modin", [18, 256])
    modG = dint("modG", [8 * 18, 256])
    if a2_only:
        qkD = nc.dram_tensor("qkD", [NSEG, 128, 2, 2, 256], BF16, kind="ExternalInput")
        kvD = nc.dram_tensor("kvD", [NCHUNK, 64, 6, 128], BF16, kind="ExternalInput")
        bgD = nc.dram_tensor("bgD", [NTOK, 16], F32, kind="ExternalInput")
    else:
        qkD = dscr("qkD", [NSEG, 128, 2, 2, 256], BF16)
        kvD = dscr("kvD", [NCHUNK, 64, 6, 128], BF16)
        bgD = dscr("bgD", [NTOK, 16])
    zD = dscr("zD", [NTOK, 512])
    oD = dscr("oD", [2, NTOK, 512])
    agin = dscr("agin", [8 * 3 * 512, 512], BF16)
    agout = dint("agout", [8 * 8 * 3 * 512 if not b_only else 1024, 512], BF16)
    x1D = dscr("x1D", [1536, D])
    if last >= 5:
        wBin = dint("wBin", [256, 5 * D]); wB = dint("wB", [D, 5 * D])
        wain = dint("wain", [512, D]); w_a_out = dint("w_a_out", [4096, D])
        wbin = dint("wbin", [256, D]); w_b_out = dint("w_b_out", [D, D])
        woin = dint("woin", [256, D]); w_o = dint("w_o", [D, D])
        wgu_b = [[dint("wgub%d%d" % (q, h), [D, D]) for h in range(2)] for q in range(4)]
        wgu_G = [[dint("wguG%d%d" % (q, h), [8 * D, D]) for h in range(2)] for q in range(4)]
        wdn_b = [dint("wdnb%d" % q, [D, D]) for q in range(4)]
        wdn_G = [dint("wdnG%d" % q, [8 * D, D]) for q in range(4)]

    def xrow(tok):
        part = tok // 4096
        w = tok % 4096
        return (w // 512) * 1536 + part * 512 + (w % 512)

    def mod_bcast(j, part):
        return bass.AP(tensor=modG, offset=(j * 6 + part) * 256, ap=[[0, 128], [18 * 256, 8], [1, 256]])

    def allgather(src_ap, dst_ap):
        return kb._emit("pool", lambda e: e.collective_compute("AllGather", ALU.bypass, replica_groups=[list(range(NCORES))],
                                                               ins=[src_ap], outs=[dst_ap]), [], [])

    with ExitStack() as st:
        kb = KB(nc, st)
        block = st.enter_context(nc.Block())
        cst = kb.sb([128, 3072], F32, name="cst")
        kb.dma("sp", cst[:], consts.ap(), writes=[cst])
        ident_f = cst[:, 0:128]
        ones_f = cst[:, 128:256]

        def mc4(d):
            return cst[0:64, 256 + d * 512: 512 + d * 512]

        def ms4(d):
            return cst[0:64, 512 + d * 512: 768 + d * 512]

        def mc1(d):
            return cst[0:64, 256 + d * 512: 320 + d * 512]

        i4 = cst[0:64, 1280:1536]
        ident_b = kb.sb([128, 128], BF16, name="ident_b")
        kb.op("dve", lambda e: e.tensor_copy(out=ident_b[:], in_=ident_f), reads=[cst], writes=[ident_b])

        class NS:
            pass
        T = NS()

        T.a2_limit = a2_limit
        import os
        if os.environ.get('KB_LIMIT'):
            kb.limit = int(os.environ['KB_LIMIT'])
        T.a2_stop = a2_stop
        T.b_groups = b_groups
        if not a2_only and not b_only:
          kb.dma("sp", xin.ap(), xb_in.ap())
          kb.dma("act", pin.ap(), posb.ap())
          kb.barrier()
          allgather(xin.ap().opt(), xG.ap().opt())
          allgather(pin.ap().opt(), pG.ap().opt())
        with ExitStack() as ps0:
          if not a2_only and not b_only:
            cTt = kb.sb([128, 48], F32, ps0)
            scT = kb.sb([128, 48], F32, ps0)
            bm = kb.sb([3, 1536], F32, ps0)
            wm = kb.sb([128, 16, 1536], F32, ps0)
            pm = [kb.ps([3, 512], F32, ps0) for _ in range(2)]
            mo = kb.sb([3, 1536], F32, ps0)
            kb.dma("sp", cTt[:], cT.ap(), writes=[cTt])
            kb.dma("sp", bm[:], b_modc.ap(), writes=[bm])
            for q in range(4):
                kb.dma("sp" if q % 2 == 0 else "act", wm[:, q * 4:(q + 1) * 4, :],
                       w_modc.ap()[q * 512:(q + 1) * 512, :].rearrange("(kt p) c -> p kt c", p=128), writes=[wm])
            kb.op("act", lambda e: e.activation(out=scT[:], in_=cTt[:], func=AF.Silu), reads=[cTt], writes=[scT])
            for ch in range(3):
                p_ = pm[ch % 2]
                for kt in range(KT):
                    kb.op("pe", lambda e, kt=kt, ch=ch, p_=p_: e.matmul(out=p_[:], lhsT=scT[:, kt * 3:(kt + 1) * 3],
                                                                      rhs=wm[:, kt, ch * 512:(ch + 1) * 512],
                                                                      start=(kt == 0), stop=(kt == KT - 1)),
                          reads=[scT, wm], writes=[p_])
                kb.op("dve", lambda e, ch=ch, p_=p_: e.tensor_tensor(out=mo[:, ch * 512:(ch + 1) * 512], in0=p_[:],
                                                                    in1=bm[:, ch * 512:(ch + 1) * 512], op=ALU.add),
                      reads=[p_, bm, mo], writes=[mo])
            kb.dma("sp", modin.ap().rearrange("(j q) i -> j q i", j=3), mo[:].rearrange("j (q i) -> j q i", q=6), reads=[mo])
            kb.barrier()
            allgather(modin.ap().opt(), modG.ap().opt())
            kb.barrier()
            kb.replay(block)
        if last >= 5 and not b_only:
            for (src_in, bounce, dst, q) in ((wB_s, wBin, wB, "sp"), (wa_s, wain, w_a_out, "act"), (wb_s, wbin, w_b_out, "sp"),
                                            (wo_s, woin, w_o, "act")):
                kb.dma(q, bounce.ap(), src_in.ap())
            for e4 in range(4):
                kb.dma("sp", wgu_b[e4][0].ap(), wgu_s.ap()[e4, :, 0:D])
                kb.dma("act", wgu_b[e4][1].ap(), wgu_s.ap()[e4, :, D:2 * D])
                kb.dma("sp", wdn_b[e4].ap(), wdn_s.ap()[e4])
            kb.barrier()
            pairs = [(wBin, wB), (wain, w_a_out), (wbin, w_b_out), (woin, w_o)]
            for e4 in range(4):
                pairs += [(wgu_b[e4][0], wgu_G[e4][0]), (wgu_b[e4][1], wgu_G[e4][1]), (wdn_b[e4], wdn_G[e4])]
            for bounce, dst in pairs:
                allgather(bounce.ap().opt(), dst.ap().opt())
        for _n, _v in list(locals().items()):
            if _n not in ("T", "NS", "st", "kb", "block"):
                setattr(T, _n, _v)
        T.mc4, T.ms4, T.mc1 = mc4, ms4, mc1
        T.lm = lambda l: cst[0:64, 1536 + l * 256:1536 + (l + 1) * 256]
        if last >= 1 and not a2_only and not b_only:
            phase_a1(kb, block, T)
        if last >= 2 and not b_only:
            phase_a2(kb, block, T)
        if last >= 3 and not b_only:
            phase_a3(kb, block, T)
        if last >= 4 and not b_only:
            kb._emit("pool", lambda e: e.collective_compute("AllGather", ALU.bypass, replica_groups=[list(range(NCORES))],
                                                             ins=[agin.ap().opt()], outs=[agout.ap().opt()]), [], [])
            kb.barrier()
            kb.replay(block)
        if last >= 5:
            phase_b(kb, block, T, moe_experts)
        kb.barrier()
        kb.replay(block)
        dbg["ninst"] = kb.ninst
    return nc, dbg


def phase_a1(kb, block, T):
    DKS = 128.0 ** -0.5
    with ExitStack() as ps:
        wA_sb = kb.sb([128, KT, WA_COLS], BF16, ps)
        for q in range(4):
            kb.dma("pool", wA_sb[:, q * 4:(q + 1) * 4, :],
                   T.wA.ap()[q * 512:(q + 1) * 512, :].rearrange("(kt p) c -> p kt c", p=128), writes=[wA_sb])
        cva = kb.sb([128, 24], F32, ps)
        dtbs = kb.sb([128, 8], F32, ps)
        nea = kb.sb([128, 8], F32, ps)
        kb.dma("sp", cva[:], T.convA.ap(), writes=[cva])
        kb.dma("sp", dtbs[:], T.dtb.ap(), writes=[dtbs])
        kb.dma("sp", nea[:], T.alog.ap(), writes=[nea])
        kb.op("act", lambda e: e.activation(out=nea[:], in_=nea[:], func=AF.Exp), reads=[nea], writes=[nea])
        kb.op("dve", lambda e: e.tensor_scalar(out=nea[:], in0=nea[:], scalar1=-1.0, scalar2=None, op0=ALU.mult),
              reads=[nea], writes=[nea])
        sc1p = kb.sb([128, D], F32, ps)
        sh1 = kb.sb([128, D], F32, ps)
        xt = [kb.sb([128, D], F32, ps) for _ in range(2)]
        pt = [kb.sb([128, D], F32, ps) for _ in range(2)]
        hb = [kb.sb([128, D], BF16, ps) for _ in range(2)]
        hT = [kb.sb([128, KT, 256], BF16, ps) for _ in range(2)]
        Pr = [kb.sb([128, 8, 258], F32, ps) for _ in range(3)]
        cv = kb.sb([128, 8, 256], F32, ps)
        sq = kb.sb([128, 4, 256], F32, ps)
        rs = kb.sb([128, 4, 256], F32, ps)
        qko = [kb.sb([128, 2, 2, 256], BF16, ps) for _ in range(2)]
        vb = kb.sb([128, 4, 256], BF16, ps)
        kvt = [kb.sb([64, 4, 6, 128], BF16, ps) for _ in range(2)]
        zs = [kb.sb([128, 512], F32, ps) for _ in range(2)]
        bgt = [kb.sb([128, 16], F32, ps) for _ in range(2)]
        ta = [kb.sb([128, 8], F32, ps) for _ in range(2)]
        tb = [kb.sb([128, 8], F32, ps) for _ in range(2)]
        pTr = kb.ps([128, KT, 128], BF16, ps)
        pP = [kb.ps([128, 4, 256], F32, ps) for _ in range(2)]
        pZ = kb.ps([128, 512], F32, ps)
        pM = kb.ps([128, 512], F32, ps)
        cst, ident_b, ones_f = T.cst, T.ident_b, T.ones_f

        def load_mod(j):
            kb.dma("sp", sc1p[:].rearrange("p (r i) -> p r i", r=8), T.mod_bcast(j, 1), writes=[sc1p])
            kb.dma("sp", sh1[:].rearrange("p (r i) -> p r i", r=8), T.mod_bcast(j, 0), writes=[sh1])
            kb.op("pool", lambda e: e.tensor_scalar(out=sc1p[:], in0=sc1p[:], scalar1=1.0, scalar2=None, op0=ALU.add),
                  reads=[sc1p], writes=[sc1p])

        def load_h(s):
            j, first, lastq, is_s, prow = seg_info(s)
            h_ = hT[s % 2]
            for t in range(2):
                tok0 = s * 256 + t * 128
                x_ = xt[t]
                xr = T.xrow(tok0)
                kb.dma("sp", x_[:], T.xG.ap()[xr:xr + 128, :], writes=[x_])
                if is_s:
                    p_ = pt[t]
                    kb.dma("act", p_[:], T.pG.ap()[prow + t * 128: prow + t * 128 + 128, :], writes=[p_])
                    kb.op("pool", lambda e, x_=x_, p_=p_: e.tensor_tensor(out=x_[:], in0=x_[:], in1=p_[:], op=ALU.add),
                          reads=[x_, p_], writes=[x_])
                kb.op("dve", lambda e, x_=x_: e.tensor_tensor(out=x_[:], in0=x_[:], in1=sc1p[:], op=ALU.mult),
                      reads=[x_, sc1p], writes=[x_])
                b_ = hb[t]
                kb.op("pool", lambda e, x_=x_, b_=b_: e.tensor_tensor(out=b_[:], in0=x_[:], in1=sh1[:], op=ALU.add),
                      reads=[x_, sh1], writes=[b_])
                for kt in range(KT):
                    kb.op("pe", lambda e, kt=kt, b_=b_: e.transpose(out=pTr[:, kt, :], in_=b_[:, kt * 128:(kt + 1) * 128],
                                                                  identity=ident_b[:]),
                          reads=[b_, ident_b], writes=[pTr])
                kb.op("act", lambda e, t=t, h_=h_: e.copy(out=h_[:, :, t * 128:(t + 1) * 128], in_=pTr[:]),
                      reads=[pTr], writes=[h_])

        def proj(s):
            j, first, lastq, is_s, prow = seg_info(s)
            P = Pr[s % 3]
            h_ = hT[s % 2]
            for half in range(2):
                pp = pP[half]
                for jj in range(4):
                    ct = half * 4 + jj
                    for kt in range(KT):
                        kb.op("pe", lambda e, jj=jj, ct=ct, kt=kt, pp=pp, h_=h_: e.matmul(
                            out=pp[:, jj, :], lhsT=wA_sb[:, kt, ct * 128:(ct + 1) * 128], rhs=h_[:, kt, :],
                            start=(kt == 0), stop=(kt == KT - 1)), reads=[wA_sb, h_], writes=[pp])
                if half == 0:
                    kb.op("act", lambda e, pp=pp, P=P: e.copy(out=P[:, 0:4, 1:257], in_=pp[:]), reads=[pp], writes=[P])
                else:
                    kb.op("dve", lambda e, pp=pp, P=P: e.tensor_copy(out=P[:, 4:8, 1:257], in_=pp[:]), reads=[pp], writes=[P])
            for t in range(2):
                tok0 = s * 256 + t * 128
                for kt in range(KT):
                    kb.op("pe", lambda e, kt=kt, t=t, h_=h_: e.matmul(out=pZ[:], lhsT=h_[:, kt, t * 128:(t + 1) * 128],
                                                                   rhs=wA_sb[:, kt, 1024:1536], start=(kt == 0), stop=(kt == KT - 1)),
                          reads=[wA_sb, h_], writes=[pZ])
                z_ = zs[t]
                kb.op("act", lambda e, z_=z_: e.activation(out=z_[:], in_=pZ[:], func=AF.Silu), reads=[pZ], writes=[z_])
                kb.dma("sp", T.zD.ap()[tok0:tok0 + 128, :], z_[:], reads=[z_])
                for kt in range(KT):
                    kb.op("pe", lambda e, kt=kt, t=t, h_=h_: e.matmul(out=pM[:, 0:16], lhsT=h_[:, kt, t * 128:(t + 1) * 128],
                                                                   rhs=wA_sb[:, kt, 1536:1552], start=(kt == 0), stop=(kt == KT - 1)),
                          reads=[wA_sb, h_], writes=[pM])
                bg = bgt[t]
                a_, b2 = ta[t], tb[t]
                kb.op("act", lambda e, bg=bg: e.activation(out=bg[:, 0:8], in_=pM[:, 0:8], func=AF.Sigmoid), reads=[pM], writes=[bg])
                kb.op("dve", lambda e, a_=a_: e.tensor_tensor(out=a_[:], in0=pM[:, 8:16], in1=dtbs[:], op=ALU.add),
                      reads=[pM, dtbs], writes=[a_])
                kb.op("dve", lambda e, a_=a_, b2=b2: e.scalar_tensor_tensor(out=b2[:], in0=a_[:], scalar=-1.0, in1=a_[:], op0=ALU.mult, op1=ALU.max),
                      reads=[a_], writes=[b2])
                kb.op("act", lambda e, b2=b2: e.activation(out=b2[:], in_=b2[:], func=AF.Exp, scale=-1.0), reads=[b2], writes=[b2])
                kb.op("act", lambda e, b2=b2: e.activation(out=b2[:], in_=b2[:], func=AF.Ln, bias=1.0), reads=[b2], writes=[b2])
                kb.op("dve", lambda e, a_=a_, b2=b2: e.scalar_tensor_tensor(out=a_[:], in0=a_[:], scalar=0.0, in1=b2[:],
                                                                          op0=ALU.max, op1=ALU.add), reads=[a_, b2], writes=[a_])
                kb.op("dve", lambda e, a_=a_, bg=bg: e.tensor_tensor(out=bg[:, 8:16], in0=a_[:], in1=nea[:], op=ALU.mult),
                      reads=[a_, nea, bg], writes=[bg])
                kb.dma("sp", T.bgD.ap()[tok0:tok0 + 128, :], bg[:], reads=[bg])
            if first:
                kb.op("pool", lambda e, P=P: e.memset(P[:, :, 0:1], 0.0), reads=[P], writes=[P])
            else:
                Pm = Pr[(s - 1) % 3]
                kb.op("pool", lambda e, P=P, Pm=Pm: e.tensor_copy(out=P[:, :, 0:1], in_=Pm[:, :, 256:257]), reads=[Pm, P], writes=[P])

        def finish(s):
            j, first, lastq, is_s, prow = seg_info(s)
            P = Pr[s % 3]
            if lastq:
                kb.op("pool", lambda e, P=P: e.memset(P[:, :, 257:258], 0.0), reads=[P], writes=[P])
            else:
                Pn = Pr[(s + 1) % 3]
                kb.op("pool", lambda e, P=P, Pn=Pn: e.tensor_copy(out=P[:, :, 257:258], in_=Pn[:, :, 1:2]), reads=[Pn, P], writes=[P])
            for ct in range(8):
                kb.op("dve", lambda e, ct=ct, P=P: e.tensor_scalar(out=cv[:, ct, :], in0=P[:, ct, 0:256], scalar1=cva[:, ct * 3:ct * 3 + 1],
                                                                  scalar2=None, op0=ALU.mult), reads=[P, cva, cv], writes=[cv])
                for k in (1, 2):
                    kb.op("dve", lambda e, ct=ct, P=P, k=k: e.scalar_tensor_tensor(
                        out=cv[:, ct, :], in0=P[:, ct, k:k + 256], scalar=cva[:, ct * 3 + k:ct * 3 + k + 1], in1=cv[:, ct, :],
                        op0=ALU.mult, op1=ALU.add), reads=[P, cva, cv], writes=[cv])
            kb.op("act", lambda e: e.activation(out=cv[:], in_=cv[:], func=AF.Silu), reads=[cv], writes=[cv])
            kb.op("pool", lambda e: e.tensor_tensor(out=sq[:], in0=cv[:, 0:4, :], in1=cv[:, 0:4, :], op=ALU.mult), reads=[cv], writes=[sq])
            for jj in range(4):
                kb.op("pe", lambda e, jj=jj: e.matmul(out=pP[0][:, jj, :], lhsT=ones_f, rhs=sq[:, jj, :], start=True, stop=True),
                      reads=[cst, sq], writes=[pP[0]])
            kb.op("act", lambda e: e.activation(out=rs[:], in_=pP[0][:], func=AF.Sqrt, bias=EPS, scale=1.0), reads=[pP[0]], writes=[rs])
            kb.op("dve", lambda e: e.reciprocal(out=rs[:], in_=rs[:]), reads=[rs], writes=[rs])
            qk = qko[s % 2]
            for kh in range(2):
                kb.op("dve", lambda e, kh=kh, qk=qk: e.scalar_tensor_tensor(out=qk[:, kh, 1, :], in0=cv[:, kh, :], scalar=DKS,
                                                                          in1=rs[:, kh, :], op0=ALU.mult, op1=ALU.mult),
                      reads=[cv, rs, qk], writes=[qk])
                kb.op("pool", lambda e, kh=kh, qk=qk: e.tensor_tensor(out=qk[:, kh, 0, :], in0=cv[:, 2 + kh, :], in1=rs[:, 2 + kh, :],
                                                                    op=ALU.mult), reads=[cv, rs, qk], writes=[qk])
            kb.op("pool", lambda e: e.tensor_copy(out=vb[:], in_=cv[:, 4:8, :]), reads=[cv], writes=[vb])
            kb.dma("sp", T.qkD.ap()[s], qk[:], reads=[qk])
            kv = kvt[s % 2]
            for c in range(4):
                for kh in range(2):
                    kb.op("pe", lambda e, kh=kh, c=c, qk=qk: e.transpose(out=pTr[0:64, kh, :], in_=qk[:, kh, 0, c * 64:(c + 1) * 64],
                                                                       identity=ident_b[:]), reads=[qk, ident_b], writes=[pTr])
                for h in range(4):
                    kb.op("pe", lambda e, h=h, c=c: e.transpose(out=pTr[0:64, 2 + h, :], in_=vb[:, h, c * 64:(c + 1) * 64],
                                                               identity=ident_b[:]), reads=[vb, ident_b], writes=[pTr])
                kb.op("act", lambda e, c=c, kv=kv: e.copy(out=kv[:, c, :, :], in_=pTr[0:64, 0:6, :]), reads=[pTr, kv], writes=[kv])
            kb.dma("sp", T.kvD.ap()[s * 4:(s + 1) * 4].rearrange("c p k d -> p c k d"), kv[:], reads=[kv])

        for s in range(NSEG + 1):
            if s < NSEG:
                if s in (0, 16, 32):
                    load_mod(seg_info(s)[0])
                load_h(s)
                proj(s)
            if s >= 1:
                finish(s - 1)
        kb.barrier()
        kb.replay(block)


QK_W = 2048
V_W = 4096
QKV_W = 8192


def _pos_table():
    quarter = D // 4
    freqs = (1.0 / (10000.0 ** (np.arange(quarter, dtype=np.float32) / np.float32(quarter)))).astype(np.float32)

    def axis_embed(n):
        ang = np.arange(n, dtype=np.float32)[:, None] * freqs[None, :]
        return np.concatenate([np.sin(ang), np.cos(ang)], axis=-1).astype(np.float32)

    er = axis_embed(64)
    ec = axis_embed(64)
    pos = np.concatenate([np.broadcast_to(er[:, None, :], (64, 64, D // 2)),
                          np.broadcast_to(ec[None, :, :], (64, 64, D // 2))], axis=-1)
    return np.ascontiguousarray(pos.reshape(4096, D).astype(np.float32))


def _consts():
    c = np.zeros((128, 3072), np.float32)
    c[:, 0:128] = np.eye(128, dtype=np.float32)
    c[:, 128:256] = 1.0
    p = np.arange(64)[:, None]
    i = np.arange(64)[None, :]
    for d in range(2):
        mc = (p <= i) if d == 0 else (p >= i)
        mc = mc.astype(np.float32)
        ms = mc - np.eye(64, dtype=np.float32)
        c[0:64, 256 + d * 512: 512 + d * 512] = np.tile(mc, (1, 4))
        c[0:64, 512 + d * 512: 768 + d * 512] = np.tile(ms, (1, 4))
    c[0:64, 1280:1536] = np.tile(np.eye(64, dtype=np.float32), (1, 4))
    for lvl in range(6):
        bsz = 1 << lvl
        lm = ((p // (2 * bsz) == i // (2 * bsz)) & (p // bsz != i // bsz)).astype(np.float32)
        c[0:64, 1536 + lvl * 256:1536 + (lvl + 1) * 256] = np.tile(lm, (1, 4))
    return c


def prep(inp, upto="all"):
    f32 = np.float32
    xall = np.concatenate([inp["x_prompt"].reshape(4096, D), inp["x_sample"].reshape(8192, D)], 0).astype(f32)
    pos = _pos_table()
    cvec = np.stack([inp["c_ctx"], inp["c"][0], inp["c"][1]], 0).astype(f32)
    cT = np.ascontiguousarray(cvec.reshape(3, KT, 128).transpose(2, 1, 0).reshape(128, 48))
    w_mod = inp["w_mod"][0]
    b_mod = inp["b_mod"][0]
    consts = _consts()
    w_in = inp["w_in"][0]
    conv_qkv = inp["conv_qkv"][0]
    full = upto in ("all", "b")
    if full:
        wB = w_in[:, QKV_W + V_W + 128:]
        convB = np.ascontiguousarray(inp["conv_b"][0].reshape(3, KT, 128).transpose(2, 1, 0).reshape(128, 48))
        lnp = np.ascontiguousarray(np.stack([np.tile(inp[k][0][None, :], (128, 1)) for k in ("ln1_g", "ln1_b", "ln2_g", "ln2_b")], 0))
        router_b = np.ascontiguousarray(np.tile(inp["router_b"][0][None, :], (128, 1)))
        bgu = np.ascontiguousarray(inp["b_gu"][0].reshape(NE, 32, 128).transpose(2, 0, 1).reshape(128, NE * 32))
    maps = []
    xs = inp["x_sample"]
    for c in range(NCORES):
        cols = []
        cblk = []
        for kh in range(2):
            cblk.append((2 * c + kh) * 128)
        for kh in range(2):
            cblk.append(QK_W + (2 * c + kh) * 128)
        for h in range(4):
            cblk.append(2 * QK_W + (4 * c + h) * 128)
        for b0 in cblk:
            cols.extend(range(b0, b0 + 128))
        for h in range(4):
            cols.extend(range(QKV_W + (4 * c + h) * 128, QKV_W + (4 * c + h) * 128 + 128))
        base_b = QKV_W + V_W
        for off in (0, 32, 64, 96):
            cols.extend(range(base_b + off + 4 * c, base_b + off + 4 * c + 4))
        wA = np.zeros((D, WA_COLS), f32)
        wA[:, :len(cols)] = w_in[:, cols]
        convA = np.zeros((128, 24), f32)
        for ct, b0 in enumerate(cblk):
            convA[:, ct * 3:(ct + 1) * 3] = conv_qkv[:, b0:b0 + 128].T
        dtv = np.concatenate([inp["dt_bias"][0][0, 4 * c:4 * c + 4], inp["dt_bias"][0][1, 4 * c:4 * c + 4]])
        alv = np.concatenate([inp["a_log"][0][0, 4 * c:4 * c + 4], inp["a_log"][0][1, 4 * c:4 * c + 4]])
        dtb = np.tile(dtv[None, :], (128, 1)).astype(f32)
        alog = np.tile(alv[None, :], (128, 1)).astype(f32)
        normw = np.tile(np.tile(inp["norm_o"][0], 4)[None, :], (128, 1)).astype(f32)
        st0 = np.stack([inp["state_fwd"][:, 0, 4 * c:4 * c + 4], inp["state_bwd"][:, 0, 4 * c:4 * c + 4]], 0).astype(f32)
        mcols = np.concatenate([np.arange(part * D + 256 * c, part * D + 256 * c + 256) for part in range(6)])
        w_modc = np.ascontiguousarray(w_mod[:, mcols])
        b_modc = np.ascontiguousarray(np.tile(b_mod[mcols][None, :], (3, 1)))
        xb = np.concatenate([xall[512 * c:512 * c + 512], xall[4096 + 512 * c:4096 + 512 * c + 512],
                             xall[8192 + 512 * c:8192 + 512 * c + 512]], 0)
        posb = pos[512 * c:512 * c + 512]
        m = dict(xb=np.ascontiguousarray(xb), posb=np.ascontiguousarray(posb), cT=cT, w_modc=w_modc, b_modc=b_modc, consts=consts,
                 wA=wA, convA=convA, dtb=dtb, alog=alog, normw=normw, st0=np.ascontiguousarray(st0))
        if full:
            xh = np.zeros((4, D), f32)
            posh = np.zeros((4, D), f32)
            hm = np.zeros((4,), f32)
            for b in range(2):
                lo = 512 * c - 1
                hi = 512 * c + 512
                if lo >= 0:
                    xh[2 * b] = xs[b, lo]
                    posh[2 * b] = pos[lo]
                    hm[2 * b] = 1.0
                if hi < 4096:
                    xh[2 * b + 1] = xs[b, hi]
                    posh[2 * b + 1] = pos[hi]
                    hm[2 * b + 1] = 1.0
            hmask = np.tile(hm[None, :], (128, 1)).astype(f32)
            idx = np.zeros((128, 96), np.int32)
            p = np.arange(128)
            for g in range(3):
                for r in range(8):
                    for ft in range(4):
                        idx[:, g * 32 + r * 4 + ft] = r * 12288 + (c * 3 + g) * 512 + ft * 128 + p
            m.update(xh=xh, posh=posh, hmask=hmask, idxg=idx, convB=convB, lnp=lnp, router_w=inp["router_w"][0], router_b=router_b,
                     bgu=bgu, b_down=inp["b_down"][0],
                     wB_s=np.ascontiguousarray(wB[256 * c:256 * c + 256]),
                     wa_s=np.ascontiguousarray(inp["w_a_out"][0][512 * c:512 * c + 512]),
                     wb_s=np.ascontiguousarray(inp["w_b_out"][0][256 * c:256 * c + 256]),
                     wo_s=np.ascontiguousarray(inp["w_o"][0][256 * c:256 * c + 256]),
                     wgu_s=np.ascontiguousarray(inp["w_gu"][0][4 * c:4 * c + 4]),
                     wdn_s=np.ascontiguousarray(inp["w_down"][0][4 * c:4 * c + 4]))
        maps.append(m)
    return maps


def phase_a2(kb, block, T):
    cst, ident_f, ones_f, i4 = T.cst, T.ident_f, T.ones_f, T.i4
    with ExitStack() as ps:
        slots = []
        for k in range(4):
            W = type("W", (), {})()
            W.qk = kb.sb([128, 2, 2, 64], BF16, ps)
            W.kv = kb.sb([64, 6, 128], BF16, ps)
            W.bg = kb.sb([64, 16], F32, ps)
            W.S = kb.sb([128, 4, 128], F32, ps)
            W.Sb = kb.sb([128, 4, 128], BF16, ps)
            W.gB = kb.sb([64, 4, 128], F32, ps)
            W.nb = kb.sb([64, 4], F32, ps)
            W.gcs = kb.sb([64, 4], F32, ps)
            W.egt = kb.sb([128, 4], F32, ps)
            W.eg = kb.sb([64, 4], F32, ps)
            W.eg2 = kb.sb([64, 4], F32, ps)
            W.GCx = kb.sb([128, 256], F32, ps)
            W.D1 = kb.sb([64, 256], F32, ps)
            W.E1 = kb.sb([64, 256], F32, ps)
            W.E1s = kb.sb([64, 256], F32, ps)
            W.AT = kb.sb([64, 256], BF16, ps)
            W.XR = kb.sb([64, 4, 128], F32, ps)
            W.Yt = kb.sb([64, 256], F32, ps)
            W.Rt = kb.sb([64, 256], F32, ps)
            W.XM = kb.sb([64, 256], F32, ps)
            W.YM = kb.sb([64, 256], F32, ps)
            W.N = kb.sb([64, 4, 128], F32, ps)
            W.Rb = kb.sb([64, 4, 64], BF16, ps)
            W.ke = kb.sb([64, 4, 128], BF16, ps)
            W.k2 = kb.sb([64, 4, 128], BF16, ps)
            W.qe = kb.sb([128, 4, 64], BF16, ps)
            W.ub = kb.sb([64, 4, 128], F32, ps)
            W.wT = kb.sb([128, 256], BF16, ps)
            W.vn = kb.sb([64, 4, 128], BF16, ps)
            W.osb = kb.sb([64, 512], F32, ps)
            W.pa = kb.ps([128, 512], F32, ps)
            W.pb = kb.ps([128, 512], F32, ps)
            slots.append(W)

        def step(W, d, gchunk, tok0, seg, off):
            pa, pb = W.pa, W.pb
            mc1 = T.mc1(d)
            kb.dma("sp", W.qk[:], T.qkD.ap()[seg, :, :, :, off:off + 64], writes=[W.qk])
            kb.dma("act", W.kv[:], T.kvD.ap()[gchunk], writes=[W.kv])
            kb.dma("sp", W.bg[:], T.bgD.ap()[tok0:tok0 + 64, :], writes=[W.bg])
            gcol = 8 + 4 * d
            bcol = 4 * d
            yield
            for h in range(4):
                kb.op("pool", lambda e, h=h: e.tensor_scalar(out=W.gB[:, h, :], in0=ones_f[0:64, :], scalar1=W.bg[:, gcol + h:gcol + h + 1],
                                                            scalar2=None, op0=ALU.mult), reads=[cst, W.bg, W.gB], writes=[W.gB])
            kb.op("pool", lambda e: e.tensor_scalar(out=W.nb[:], in0=W.bg[:, bcol:bcol + 4], scalar1=-1.0, scalar2=None, op0=ALU.mult),
                  reads=[W.bg], writes=[W.nb])
            kb.op("pe", lambda e: e.matmul(out=pa[0:64, 0:4], lhsT=mc1, rhs=W.bg[:, gcol:gcol + 4], start=True, stop=True),
                  reads=[cst, W.bg], writes=[pa])
            kb.op("pe", lambda e: e.matmul(out=pa[:, 8:12], lhsT=ones_f[0:64, :], rhs=W.bg[:, gcol:gcol + 4], start=True, stop=True),
                  reads=[cst, W.bg], writes=[pa])
            for h in range(4):
                kb.op("pe", lambda e, h=h: e.matmul(out=pb[:, h * 64:(h + 1) * 64], lhsT=W.gB[:, h, :], rhs=mc1, start=True, stop=True),
                      reads=[W.gB, cst], writes=[pb])
            yield
            kb.op("dve", lambda e: e.tensor_copy(out=W.gcs[:], in_=pa[0:64, 0:4]), reads=[pa], writes=[W.gcs])
            kb.op("act", lambda e: e.activation(out=W.egt[:], in_=pa[:, 8:12], func=AF.Exp), reads=[pa], writes=[W.egt])
            kb.op("dve", lambda e: e.tensor_tensor(out=W.eg2[:], in0=pa[0:64, 8:12], in1=W.gcs[:], op=ALU.subtract),
                  reads=[pa, W.gcs], writes=[W.eg2])
            kb.op("act", lambda e: e.activation(out=W.eg2[:], in_=W.eg2[:], func=AF.Exp), reads=[W.eg2], writes=[W.eg2])
            kb.op("act", lambda e: e.activation(out=W.eg[:], in_=W.gcs[:], func=AF.Exp), reads=[W.gcs], writes=[W.eg])
            kb.op("act", lambda e: e.activation(out=W.GCx[:], in_=pb[:, 0:256], func=AF.Exp), reads=[pb], writes=[W.GCx])
            for h in range(4):
                kb.op("act", lambda e, h=h: e.activation(out=W.D1[:, h * 64:(h + 1) * 64], in_=pb[0:64, h * 64:(h + 1) * 64],
                                                        func=AF.Relu, bias=W.gcs[:, h:h + 1], scale=-1.0),
                      reads=[pb, W.gcs, W.D1], writes=[W.D1])
            kb.op("act", lambda e: e.activation(out=W.D1[:], in_=W.D1[:], func=AF.Exp, scale=-1.0), reads=[W.D1], writes=[W.D1])
            kb.op("dve", lambda e: e.tensor_tensor(out=W.E1[:], in0=W.D1[:], in1=T.mc4(d), op=ALU.mult), reads=[W.D1, cst], writes=[W.E1])
            kb.op("pool", lambda e: e.tensor_tensor(out=W.E1s[:], in0=W.D1[:], in1=T.ms4(d), op=ALU.mult), reads=[W.D1, cst], writes=[W.E1s])
            for kh in range(2):
                kb.op("pe", lambda e, kh=kh: e.matmul(out=pa[0:64, kh * 128:(kh + 1) * 128], lhsT=W.qk[:, kh, 0, :], rhs=W.qk[:, kh, :, :],
                                                     start=True, stop=True), reads=[W.qk], writes=[pa])
            yield
            for h in range(4):
                kh = h // 2
                kb.op("dve", lambda e, h=h, kh=kh: e.tensor_tensor(out=W.AT[:, h * 64:(h + 1) * 64], in0=pa[0:64, kh * 128 + 64:kh * 128 + 128],
                                                                  in1=W.E1[:, h * 64:(h + 1) * 64], op=ALU.mult),
                      reads=[pa, W.E1, W.AT], writes=[W.AT])
                kb.op("dve", lambda e, h=h, kh=kh: e.scalar_tensor_tensor(out=W.XR[:, h, 0:64], in0=pa[0:64, kh * 128:kh * 128 + 64],
                                                                         scalar=W.bg[:, bcol + h:bcol + h + 1],
                                                                         in1=W.E1s[:, h * 64:(h + 1) * 64], op0=ALU.mult, op1=ALU.mult),
                      reads=[pa, W.bg, W.E1s, W.XR], writes=[W.XR])
            i4v = i4.rearrange("p (h i) -> p h i", h=4)
            kb.op("pool", lambda e: e.tensor_copy(out=W.XR[:, :, 64:128], in_=i4v), reads=[cst, W.XR], writes=[W.XR])
            kb.op("pool", lambda e: e.tensor_copy(out=W.Rt[:], in_=i4), reads=[cst], writes=[W.Rt])
            for h in range(4):
                kb.op("pe", lambda e, h=h: e.matmul(out=pb[0:64, h * 64:(h + 1) * 64], lhsT=W.XR[:, h, 0:64], rhs=ident_f[0:64, 0:64],
                                                   start=True, stop=True), reads=[W.XR, cst], writes=[pb])
            yield
            kb.op("act", lambda e: e.copy(out=W.Yt[:], in_=pb[0:64, 0:256]), reads=[pb], writes=[W.Yt])
            pa3 = pa[0:64, :].rearrange("p (h c) -> p h c", h=4)
            for lvl in range(6):
                lm = T.lm(lvl)
                kb.op("dve", lambda e, lm=lm: e.tensor_tensor(out=W.XM[:].rearrange("p (h i) -> p h i", h=4), in0=W.XR[:, :, 0:64],
                                                              in1=lm.rearrange("p (h i) -> p h i", h=4), op=ALU.mult),
                      reads=[W.XR, cst], writes=[W.XM])
                kb.op("pool", lambda e, lm=lm: e.tensor_tensor(out=W.YM[:], in0=W.Yt[:], in1=lm, op=ALU.mult), reads=[W.Yt, cst], writes=[W.YM])
                for h in range(4):
                    kb.op("pe", lambda e, h=h: e.matmul(out=pa[0:64, h * 128:h * 128 + 64], lhsT=W.YM[:, h * 64:(h + 1) * 64],
                                                       rhs=W.XR[:, h, 64:128], start=True, stop=True), reads=[W.YM, W.XR], writes=[pa])
                    kb.op("pe", lambda e, h=h: e.matmul(out=pa[0:64, h * 128 + 64:h * 128 + 128], lhsT=W.XM[:, h * 64:(h + 1) * 64],
                                                       rhs=W.Rt[:, h * 64:(h + 1) * 64], start=True, stop=True), reads=[W.XM, W.Rt], writes=[pa])
                yield
                kb.op("act", lambda e: e.copy(out=W.N[:], in_=pa3), reads=[pa], writes=[W.N])
                for h in range(4):
                    kb.op("pe", lambda e, h=h: e.matmul(out=pb[0:64, h * 64:(h + 1) * 64], lhsT=W.Rt[:, h * 64:(h + 1) * 64],
                                                       rhs=W.N[:, h, 0:64], start=True, stop=True), reads=[W.Rt, W.N], writes=[pb])
                    kb.op("pe", lambda e, h=h: e.matmul(out=pa[0:64, h * 128:h * 128 + 64], lhsT=W.XR[:, h, 64:128],
                                                       rhs=W.N[:, h, 64:128], start=True, stop=True), reads=[W.XR, W.N], writes=[pa])
                yield
                kb.op("dve", lambda e: e.tensor_tensor(out=W.XR[:, :, 64:128], in0=W.XR[:, :, 64:128],
                                                       in1=pb[0:64, 0:256].rearrange("p (h i) -> p h i", h=4), op=ALU.subtract),
                      reads=[pb, W.XR], writes=[W.XR])
                kb.op("dve", lambda e: e.tensor_tensor(out=W.Rt[:].rearrange("p (h i) -> p h i", h=4), in0=W.Rt[:].rearrange("p (h i) -> p h i", h=4),
                                                       in1=pa3[:, :, 0:64], op=ALU.subtract), reads=[pa, W.Rt], writes=[W.Rt])
            kb.op("act", lambda e: e.copy(out=W.Rb[:], in_=W.XR[:, :, 64:128]), reads=[W.XR], writes=[W.Rb])
            for h in range(4):
                kh = h // 2
                kb.op("pool", lambda e, h=h, kh=kh: e.tensor_scalar(out=W.ke[:, h, :], in0=W.kv[:, kh, :], scalar1=W.eg[:, h:h + 1],
                                                                   scalar2=None, op0=ALU.mult), reads=[W.kv, W.eg, W.ke], writes=[W.ke])
                kb.op("pool", lambda e, h=h, kh=kh: e.tensor_scalar(out=W.k2[:, h, :], in0=W.kv[:, kh, :], scalar1=W.eg2[:, h:h + 1],
                                                                   scalar2=None, op0=ALU.mult), reads=[W.kv, W.eg2, W.k2], writes=[W.k2])
                kb.op("dve", lambda e, h=h, kh=kh: e.tensor_tensor(out=W.qe[:, h, :], in0=W.qk[:, kh, 1, :], in1=W.GCx[:, h * 64:(h + 1) * 64],
                                                                  op=ALU.mult), reads=[W.qk, W.GCx, W.qe], writes=[W.qe])
            for h in range(4):
                kb.op("pe", lambda e, h=h: e.matmul(out=pa[0:64, h * 128:(h + 1) * 128], lhsT=W.Rb[:, h, :], rhs=W.kv[:, 2 + h, :],
                                                   start=True, stop=True), reads=[W.Rb, W.kv], writes=[pa])
            for h in range(4):
                kb.op("pe", lambda e, h=h: e.matmul(out=pb[:, h * 64:(h + 1) * 64], lhsT=W.ke[:, h, :], rhs=W.Rb[:, h, :],
                                                   start=True, stop=True), reads=[W.Rb, W.ke], writes=[pb])
            yield
            for h in range(4):
                kb.op("dve", lambda e, h=h: e.tensor_scalar(out=W.ub[:, h, :], in0=pa[0:64, h * 128:(h + 1) * 128],
                                                           scalar1=W.bg[:, bcol + h:bcol + h + 1], scalar2=None, op0=ALU.mult),
                      reads=[pa, W.bg, W.ub], writes=[W.ub])
            kb.op("act", lambda e: e.copy(out=W.wT[:], in_=pb[:, 0:256]), reads=[pb], writes=[W.wT])
            for h in range(4):
                kb.op("pe", lambda e, h=h: e.matmul(out=pa[0:64, h * 128:(h + 1) * 128], lhsT=W.wT[:, h * 64:(h + 1) * 64], rhs=W.Sb[:, h, :],
                                                   start=True, stop=True), reads=[W.wT, W.Sb], writes=[pa])
            yield
            for h in range(4):
                kb.op("dve", lambda e, h=h: e.scalar_tensor_tensor(out=W.vn[:, h, :], in0=pa[0:64, h * 128:(h + 1) * 128],
                                                                  scalar=W.nb[:, h:h + 1], in1=W.ub[:, h, :], op0=ALU.mult, op1=ALU.add),
                      reads=[pa, W.nb, W.ub, W.vn], writes=[W.vn])
            for h in range(4):
                kb.op("pe", lambda e, h=h: e.matmul(out=pb[0:64, h * 128:(h + 1) * 128], lhsT=W.qe[:, h, :], rhs=W.Sb[:, h, :],
                                                   start=True, stop=False), reads=[W.qe, W.Sb], writes=[pb])
                kb.op("pe", lambda e, h=h: e.matmul(out=pb[0:64, h * 128:(h + 1) * 128], lhsT=W.AT[:, h * 64:(h + 1) * 64], rhs=W.vn[:, h, :],
                                                   start=False, stop=True), reads=[W.AT, W.vn], writes=[pb])
            for h in range(4):
                kb.op("pe", lambda e, h=h: e.matmul(out=pa[:, h * 128:(h + 1) * 128], lhsT=W.k2[:, h, :], rhs=W.vn[:, h, :],
                                                   start=True, stop=True), reads=[W.k2, W.vn], writes=[pa])
            yield
            kb.op("act", lambda e: e.copy(out=W.osb[:], in_=pb[0:64, :]), reads=[pb], writes=[W.osb])
            kb.dma("sp", T.oD.ap()[d, tok0:tok0 + 64, :], W.osb[:], reads=[W.osb])
            for h in range(4):
                kb.op("dve", lambda e, h=h: e.scalar_tensor_tensor(out=W.S[:, h, :], in0=W.S[:, h, :], scalar=W.egt[:, h:h + 1],
                                                                  in1=pa[:, h * 128:(h + 1) * 128], op0=ALU.mult, op1=ALU.add),
                      reads=[pa, W.egt, W.S], writes=[W.S])
            kb.op("act", lambda e: e.copy(out=W.Sb[:], in_=W.S[:]), reads=[W.S], writes=[W.Sb])
            yield

        def run_stream(W, kind, idx, d):
            if kind == "p":
                n = 4
                cbase = idx * 4
                kb.op("pool", lambda e: e.memset(W.S[:], 0.0), reads=[W.S], writes=[W.S])
            else:
                n = 64
                cbase = 64 + idx * 64
                kb.dma("sp", W.S[:], T.st0.ap()[d, idx].rearrange("h k v -> k h v"), writes=[W.S])
            kb.op("act", lambda e: e.copy(out=W.Sb[:], in_=W.S[:]), reads=[W.S], writes=[W.Sb])
            for i in range(n):
                c = i if d == 0 else n - 1 - i
                gchunk = cbase + c
                stop = getattr(T, "a2_stop", None)
                for n_y, _ in enumerate(step(W, d, gchunk, gchunk * 64, gchunk // 4, (gchunk % 4) * 64)):
                    if stop is not None and n_y + 1 >= stop:
                        break
                    yield
            if kind == "p":
                kb.dma("sp", T.ns_out.ap()[d, idx].rearrange("h k v -> k h v"), W.S[:], reads=[W.S])
                yield

        def slot_gen(k):
            W = slots[k]
            lim = getattr(T, "a2_limit", None)
            for n_, i in enumerate(range(k, 32, 4)):
                if lim is not None and n_ >= lim:
                    break
                yield from run_stream(W, "p", i // 2, i % 2)
            if lim is None:
                yield from run_stream(W, "s", k // 2, k % 2)

        gens = [slot_gen(k) for k in range(4)]
        active = list(gens)
        while active:
            for g in list(active):
                try:
                    next(g)
                except StopIteration:
                    active.remove(g)
        kb.barrier()
        kb.replay(block)


def phase_a3(kb, block, T):
    ident_b = T.ident_b
    with ExitStack() as ps:
        nw = kb.sb([128, 512], F32, ps)
        kb.dma("sp", nw[:], T.normw.ap(), writes=[nw])
        of = [kb.sb([128, 512], F32, ps) for _ in range(2)]
        ob = [kb.sb([128, 512], F32, ps) for _ in range(2)]
        zt = [kb.sb([128, 512], F32, ps) for _ in range(2)]
        sqt = [kb.sb([128, 512], F32, ps) for _ in range(2)]
        ss = [kb.sb([128, 4], F32, ps) for _ in range(2)]
        og = [kb.sb([128, 512], F32, ps) for _ in range(2)]
        ogb = [kb.sb([128, 512], BF16, ps) for _ in range(2)]
        ogT = [kb.sb([128, 4, 128], BF16, ps) for _ in range(2)]
        pT = [kb.ps([128, 4, 128], BF16, ps) for _ in range(2)]
        agv = T.agin.ap().rearrange("(j g h p) t -> j g p h t", j=8, g=3, h=4, p=128)
        for tt in range(96):
            i = tt % 2
            o_, b_, z_, q_, s_, g_, gb_, gT_, p_ = of[i], ob[i], zt[i], sqt[i], ss[i], og[i], ogb[i], ogT[i], pT[i]
            kb.dma("sp", o_[:], T.oD.ap()[0, tt * 128:(tt + 1) * 128, :], writes=[o_])
            kb.dma("act", b_[:], T.oD.ap()[1, tt * 128:(tt + 1) * 128, :], writes=[b_])
            kb.dma("sp", z_[:], T.zD.ap()[tt * 128:(tt + 1) * 128, :], writes=[z_])
            kb.op("dve", lambda e, o_=o_, b_=b_: e.tensor_tensor(out=o_[:], in0=o_[:], in1=b_[:], op=ALU.add), reads=[o_, b_], writes=[o_])
            kb.op("pool", lambda e, o_=o_, q_=q_: e.tensor_tensor(out=q_[:], in0=o_[:], in1=o_[:], op=ALU.mult), reads=[o_], writes=[q_])
            kb.op("dve", lambda e, q_=q_, s_=s_: e.reduce_sum(out=s_[:], in_=q_[:].rearrange("p (h d) -> p h d", h=4), axis=AX.X),
                  reads=[q_], writes=[s_])
            kb.op("act", lambda e, s_=s_: e.activation(out=s_[:], in_=s_[:], func=AF.Sqrt, bias=EPS, scale=1.0 / 128.0), reads=[s_], writes=[s_])
            kb.op("dve", lambda e, s_=s_: e.reciprocal(out=s_[:], in_=s_[:]), reads=[s_], writes=[s_])
            for h in range(4):
                kb.op("dve", lambda e, h=h, o_=o_, g_=g_, s_=s_: e.tensor_scalar(out=g_[:, h * 128:(h + 1) * 128], in0=o_[:, h * 128:(h + 1) * 128],
                                                                               scalar1=s_[:, h:h + 1], scalar2=None, op0=ALU.mult),
                      reads=[o_, s_, g_], writes=[g_])
            kb.op("pool", lambda e, g_=g_: e.tensor_tensor(out=g_[:], in0=g_[:], in1=nw[:], op=ALU.mult), reads=[g_, nw], writes=[g_])
            kb.op("pool", lambda e, g_=g_, z_=z_, gb_=gb_: e.tensor_tensor(out=gb_[:], in0=g_[:], in1=z_[:], op=ALU.mult), reads=[g_, z_], writes=[gb_])
            for h in range(4):
                kb.op("pe", lambda e, h=h, gb_=gb_, p_=p_: e.transpose(out=p_[:, h, :], in_=gb_[:, h * 128:(h + 1) * 128], identity=ident_b[:]),
                      reads=[gb_, ident_b], writes=[p_])
            kb.op("act", lambda e, p_=p_, gT_=gT_: e.copy(out=gT_[:], in_=p_[:]), reads=[p_], writes=[gT_])
            j = (tt % 32) // 4
            part = tt // 32
            lt = tt % 4
            kb.dma("sp", agv[j, part, :, :, lt * 128:(lt + 1) * 128], gT_[:], reads=[gT_])
        kb.barrier()
        kb.replay(block)


def phase_b(kb, block, T, nexp):
    cst, ident_f, ident_b = T.cst, T.ident_f, T.ident_b

    def bview(ap2d, kt):
        return ap2d.rearrange("p (kt c) -> p kt c", kt=kt)

    with ExitStack() as pbs:
        cvb = kb.sb([128, 48], F32, pbs)
        hm = kb.sb([128, 4], F32, pbs)
        idx = [kb.sb([128, 1], I32, pbs) for _ in range(96)]
        rbt = kb.sb([128, 32], F32, pbs)
        bgu_t = kb.sb([128, 1024], F32, pbs)
        rw = kb.sb([128, 16, 32], F32, pbs)
        bdn = kb.sb([32, 2048], F32, pbs)
        Gall = kb.sb([128, 12, 32], F32, pbs)
        stats = kb.sb([128, 24], F32, pbs)
        mv = kb.sb([128, 2], F32, pbs)
        rstd = kb.sb([128, 1], F32, pbs)
        nmr = kb.sb([128, 1], F32, pbs)
        kb.dma("sp", cvb[:], T.convB.ap(), writes=[cvb])
        kb.dma("sp", hm[:], T.hmask.ap(), writes=[hm])
        for k_ in range(96):
            kb.dma("sp" if k_ % 2 == 0 else "act", idx[k_][:], T.idxg.ap()[:, k_:k_ + 1], writes=[idx[k_]], allow_slow_non_contiguous=True)
        kb.dma("sp", rbt[:], T.router_b.ap(), writes=[rbt])
        kb.dma("sp", bgu_t[:], T.bgu.ap(), writes=[bgu_t])
        kb.dma("sp", rw[:], T.router_w.ap().rearrange("(kt p) e -> p kt e", p=128), writes=[rw])
        kb.dma("sp", bdn[:], T.b_down.ap(), writes=[bdn])

        def layernorm(src, gam, bet, dst):
            for cc in range(4):
                kb.op("dve", lambda e, cc=cc: e.bn_stats(out=stats[:, cc * 6:(cc + 1) * 6], in_=src[:, cc * 512:(cc + 1) * 512]),
                      reads=[src, stats], writes=[stats])
            kb.op("dve", lambda e: e.bn_aggr(out=mv[:], in_=stats[:]), reads=[stats], writes=[mv])
            kb.op("act", lambda e: e.activation(out=rstd[:], in_=mv[:, 1:2], func=AF.Sqrt, bias=EPS, scale=1.0), reads=[mv], writes=[rstd])
            kb.op("dve", lambda e: e.reciprocal(out=rstd[:], in_=rstd[:]), reads=[rstd], writes=[rstd])
            kb.op("dve", lambda e: e.scalar_tensor_tensor(out=nmr[:], in0=mv[:, 0:1], scalar=-1.0, in1=rstd[:], op0=ALU.mult, op1=ALU.mult),
                  reads=[mv, rstd], writes=[nmr])
            kb.op("act", lambda e: e.activation(out=dst[:], in_=src[:], func=AF.Identity, bias=nmr[:, 0:1], scale=rstd[:, 0:1]),
                  reads=[src, nmr, rstd], writes=[dst])
            kb.op("pool", lambda e: e.tensor_tensor(out=dst[:], in0=dst[:], in1=gam[:], op=ALU.mult), reads=[dst, gam], writes=[dst])
            kb.op("pool", lambda e: e.tensor_tensor(out=dst[:], in0=dst[:], in1=bet[:], op=ALU.add), reads=[dst, bet], writes=[dst])

        def load_x(g, t, x_, p_):
            kb.dma("sp", x_[:], T.xb_in.ap()[(g * 4 + t) * 128:(g * 4 + t + 1) * 128, :], writes=[x_])
            if g >= 1:
                kb.dma("act", p_[:], T.posb.ap()[t * 128:(t + 1) * 128, :], writes=[p_])
                kb.op("pool", lambda e: e.tensor_tensor(out=x_[:], in0=x_[:], in1=p_[:], op=ALU.add), reads=[x_, p_], writes=[x_])

        def modload(tl, g, part, plus1=False):
            kb.dma("sp", tl[:].rearrange("p (r i) -> p r i", r=8), T.mod_bcast(g, part), writes=[tl])
            if plus1:
                kb.op("pool", lambda e: e.tensor_scalar(out=tl[:], in0=tl[:], scalar1=1.0, scalar2=None, op0=ALU.add), reads=[tl], writes=[tl])

        for g in T.b_groups:
            with ExitStack() as pg:
                hT = kb.sb([128, 16, 512], BF16, pg)
                mT = kb.sb([128, 16, 512], BF16, pg)
                wch = [kb.sb([128, 8192], BF16, pg) for _ in range(2)]
                with ExitStack() as pab:
                    U1 = kb.sb([128, 16, 512], BF16, pab)
                    U2 = kb.sb([128, 16, 512], BF16, pab)
                    U4 = kb.sb([128, 16, 512], BF16, pab)
                    cx = kb.sb([128, 16, 516], BF16, pab)
                    uch = kb.sb([128, 16, 2], F32, pab)
                    cxh = kb.sb([128, 2], F32, pab)
                    kb.op("pool", lambda e: e.memset(cx[:], 0.0), writes=[cx])
                    with ExitStack() as pa_:
                        sc1p = kb.sb([128, D], F32, pa_)
                        sh1 = kb.sb([128, D], F32, pa_)
                        XT = [kb.sb([128, D], F32, pa_) for _ in range(2)]
                        PT1 = kb.sb([128, D], F32, pa_)
                        PT = [PT1, PT1]
                        hb = kb.sb([128, D], BF16, pa_)
                        hTh = kb.sb([128, 16, 2], BF16, pa_)
                        pTr = kb.ps([128, 16, 128], BF16, pa_)
                        pQ = [kb.ps([128, 512], F32, pa_) for _ in range(4)]
                        pH = kb.ps([128, 8], F32, pa_)
                        modload(sc1p, g, 1, True)
                        modload(sh1, g, 0)
                        for t in range(4):
                            x_ = XT[t % 2]
                            load_x(g, t, x_, PT[t % 2])
                            kb.op("dve", lambda e, x_=x_: e.tensor_tensor(out=x_[:], in0=x_[:], in1=sc1p[:], op=ALU.mult), reads=[x_, sc1p], writes=[x_])
                            kb.op("pool", lambda e, x_=x_: e.tensor_tensor(out=hb[:], in0=x_[:], in1=sh1[:], op=ALU.add), reads=[x_, sh1], writes=[hb])
                            for kt in range(KT):
                                kb.op("pe", lambda e, kt=kt: e.transpose(out=pTr[:, kt, :], in_=hb[:, kt * 128:(kt + 1) * 128], identity=ident_b[:]),
                                      reads=[hb, ident_b], writes=[pTr])
                            kb.op("act", lambda e, t=t: e.copy(out=hT[:, :, t * 128:(t + 1) * 128], in_=pTr[:]), reads=[pTr], writes=[hT])
                        if g >= 1:
                            xh_t, ph_t = XT[0], PT1
                            kb.dma("sp", xh_t[0:2, :], T.xh.ap()[(g - 1) * 2:(g - 1) * 2 + 2, :], writes=[xh_t])
                            kb.dma("sp", ph_t[0:2, :], T.posh.ap()[(g - 1) * 2:(g - 1) * 2 + 2, :], writes=[ph_t])
                            kb.op("dve", lambda e: e.tensor_tensor(out=xh_t[0:2, :], in0=xh_t[0:2, :], in1=ph_t[0:2, :], op=ALU.add), reads=[xh_t, ph_t], writes=[xh_t])
                            kb.op("dve", lambda e: e.tensor_tensor(out=xh_t[0:2, :], in0=xh_t[0:2, :], in1=sc1p[0:2, :], op=ALU.mult), reads=[xh_t, sc1p], writes=[xh_t])
                            kb.op("dve", lambda e: e.tensor_tensor(out=hb[0:2, :], in0=xh_t[0:2, :], in1=sh1[0:2, :], op=ALU.add), reads=[xh_t, sh1, hb], writes=[hb])
                            for kt in range(KT):
                                kb.op("pe", lambda e, kt=kt: e.transpose(out=pTr[:, kt, 0:2], in_=hb[0:2, kt * 128:(kt + 1) * 128],
                                                                       identity=ident_b[0:2, 0:2]), reads=[hb, ident_b], writes=[pTr])
                            kb.op("act", lambda e: e.copy(out=hTh[:], in_=pTr[:, :, 0:2]), reads=[pTr], writes=[hTh])

                        def wloadB(cc):
                            w = wch[cc % 2]
                            kb.dma("pool", bview(w[:, 0:4096], 16), T.wB.ap()[:, cc * 256:(cc + 1) * 256].rearrange("(kt p) c -> p kt c", p=128),
                                   writes=[w])
                        wloadB(0)
                        for cc in range(40):
                            if cc + 1 < 40:
                                wloadB(cc + 1)
                            w = wch[cc % 2]
                            wv = bview(w[:, 0:4096], 16)
                            sec = cc // 8
                            for j in range(2):
                                fi = (cc % 8) * 2 + j
                                pq = pQ[(cc * 2 + j) % 4]
                                for kt in range(KT):
                                    kb.op("pe", lambda e, kt=kt, j=j, wv=wv, pq=pq: e.matmul(out=pq[:], lhsT=wv[:, kt, j * 128:(j + 1) * 128],
                                                                                           rhs=hT[:, kt, :], start=(kt == 0), stop=(kt == KT - 1)),
                                          reads=[w, hT], writes=[pq])
                                halo = g >= 1 and sec in (1, 2)
                                if halo:
                                    for kt in range(KT):
                                        kb.op("pe", lambda e, kt=kt, j=j, wv=wv: e.matmul(out=pH[:, 0:2], lhsT=wv[:, kt, j * 128:(j + 1) * 128],
                                                                                        rhs=hTh[:, kt, :], start=(kt == 0), stop=(kt == KT - 1)),
                                              reads=[w, hTh], writes=[pH])
                                if sec == 0:
                                    kb.op("act", lambda e, fi=fi, pq=pq: e.copy(out=U1[:, fi, :], in_=pq[:]), reads=[pq, U1], writes=[U1])
                                elif sec == 1:
                                    kb.op("act", lambda e, fi=fi, pq=pq: e.copy(out=U2[:, fi, :], in_=pq[:]), reads=[pq, U2], writes=[U2])
                                    if halo:
                                        kb.op("dve", lambda e, fi=fi: e.tensor_copy(out=uch[:, fi, :], in_=pH[:, 0:2]), reads=[pH, uch], writes=[uch])
                                elif sec == 2:
                                    if g == 0:
                                        for (c0, t0) in ((1, 0), (259, 256)):
                                            kb.op("dve", lambda e, fi=fi, pq=pq, c0=c0, t0=t0: e.tensor_tensor(
                                                out=cx[:, fi, c0:c0 + 256], in0=pq[:, t0:t0 + 256], in1=U2[:, fi, t0:t0 + 256], op=ALU.mult),
                                                reads=[pq, U2, cx], writes=[cx])
                                    else:
                                        kb.op("dve", lambda e, fi=fi, pq=pq: e.tensor_tensor(out=cx[:, fi, 1:513], in0=pq[:], in1=U2[:, fi, :], op=ALU.mult),
                                              reads=[pq, U2, cx], writes=[cx])
                                        kb.op("dve", lambda e, fi=fi: e.tensor_tensor(out=cxh[:], in0=pH[:, 0:2], in1=uch[:, fi, :], op=ALU.mult),
                                              reads=[pH, uch], writes=[cxh])
                                        m0 = 2 * (g - 1)
                                        kb.op("dve", lambda e, fi=fi, m0=m0: e.tensor_tensor(out=cx[:, fi, 0:1], in0=cxh[:, 0:1], in1=hm[:, m0:m0 + 1], op=ALU.mult),
                                              reads=[cxh, hm, cx], writes=[cx])
                                        kb.op("dve", lambda e, fi=fi, m0=m0: e.tensor_tensor(out=cx[:, fi, 513:514], in0=cxh[:, 1:2], in1=hm[:, m0 + 1:m0 + 2],
                                                                                           op=ALU.mult), reads=[cxh, hm, cx], writes=[cx])
                                elif sec == 3:
                                    kb.op("act", lambda e, fi=fi, pq=pq: e.activation(out=mT[:, fi, :], in_=pq[:], func=AF.Sigmoid), reads=[pq, mT], writes=[mT])
                                else:
                                    kb.op("act", lambda e, fi=fi, pq=pq: e.activation(out=U4[:, fi, :], in_=pq[:], func=AF.Sigmoid), reads=[pq, U4], writes=[U4])
                        kb.barrier()
                        kb.replay(block)
                    with ExitStack() as pbeta:
                        ogT = kb.sb([128, 32, 512], BF16, pbeta)
                        tmp = [kb.sb([128, 512], F32, pbeta) for _ in range(2)]
                        tmp2 = [kb.sb([128, 512], F32, pbeta) for _ in range(2)]
                        pY = [kb.ps([128, 512], F32, pbeta) for _ in range(4)]
                        for kt in range(32):
                            kb._emit("pool", lambda e, kt=kt: e.indirect_dma_start(
                                out=ogT[:, kt, :], out_offset=None, in_=T.agout.ap(),
                                in_offset=bass.IndirectOffsetOnAxis(ap=idx[g * 32 + kt][:, :], axis=0)),
                                     [idx[g * 32 + kt]], [ogT], dma=True)
                        segs = [(1, 0, 256), (259, 256, 256)] if g == 0 else [(1, 0, 512)]
                        for fi in range(16):
                            tm = tmp[fi % 2]
                            for (c0, t0, n) in segs:
                                kb.op("dve", lambda e, fi=fi, c0=c0, n=n, tm=tm: e.tensor_scalar(out=tm[:, 0:n], in0=cx[:, fi, c0 - 1:c0 - 1 + n],
                                                                                               scalar1=cvb[:, fi * 3:fi * 3 + 1], scalar2=None, op0=ALU.mult),
                                      reads=[cx, cvb, tm], writes=[tm])
                                for k in (1, 2):
                                    kb.op("dve", lambda e, fi=fi, c0=c0, n=n, tm=tm, k=k: e.scalar_tensor_tensor(
                                        out=tm[:, 0:n], in0=cx[:, fi, c0 - 1 + k:c0 - 1 + k + n], scalar=cvb[:, fi * 3 + k:fi * 3 + k + 1],
                                        in1=tm[:, 0:n], op0=ALU.mult, op1=ALU.add), reads=[cx, cvb, tm], writes=[tm])
                                kb.op("pool", lambda e, fi=fi, t0=t0, n=n, tm=tm: e.tensor_tensor(out=U2[:, fi, t0:t0 + n], in0=tm[:, 0:n],
                                                                                                in1=U1[:, fi, t0:t0 + n], op=ALU.mult),
                                      reads=[tm, U1, U2], writes=[U2])

                        def wloadY(ot):
                            w = wch[ot % 2]
                            kb.dma("pool", bview(w[:, 0:4096], 32), T.w_a_out.ap()[:, ot * 128:(ot + 1) * 128].rearrange("(kt p) c -> p kt c", p=128),
                                   writes=[w])
                            kb.dma("pool", bview(w[:, 4096:6144], 16), T.w_b_out.ap()[:, ot * 128:(ot + 1) * 128].rearrange("(kt p) c -> p kt c", p=128),
                                   writes=[w])
                        wloadY(0)
                        for ot in range(16):
                            if ot + 1 < 16:
                                wloadY(ot + 1)
                            w = wch[ot % 2]
                            wa = bview(w[:, 0:4096], 32)
                            wb = bview(w[:, 4096:6144], 16)
                            pya = pY[(2 * ot) % 4]
                            pyb = pY[(2 * ot + 1) % 4]
                            for kt in range(32):
                                kb.op("pe", lambda e, kt=kt, wa=wa, pya=pya: e.matmul(out=pya[:], lhsT=wa[:, kt, :], rhs=ogT[:, kt, :],
                                                                                    start=(kt == 0), stop=(kt == 31)), reads=[w, ogT], writes=[pya])
                            for kt in range(16):
                                kb.op("pe", lambda e, kt=kt, wb=wb, pyb=pyb: e.matmul(out=pyb[:], lhsT=wb[:, kt, :], rhs=U2[:, kt, :],
                                                                                    start=(kt == 0), stop=(kt == 15)), reads=[w, U2], writes=[pyb])
                            ta_, tb_ = tmp[ot % 2], tmp2[ot % 2]
                            kb.op("dve", lambda e, ot=ot, pya=pya, ta_=ta_: e.tensor_tensor(out=ta_[:], in0=pya[:], in1=mT[:, ot, :], op=ALU.mult),
                                  reads=[pya, mT], writes=[ta_])
                            kb.op("dve", lambda e, ot=ot, pyb=pyb, tb_=tb_: e.tensor_tensor(out=tb_[:], in0=pyb[:], in1=U4[:, ot, :], op=ALU.mult),
                                  reads=[pyb, U4], writes=[tb_])
                            kb.op("pool", lambda e, ot=ot, ta_=ta_, tb_=tb_: e.tensor_tensor(out=mT[:, ot, :], in0=ta_[:], in1=tb_[:], op=ALU.add),
                                  reads=[ta_, tb_, mT], writes=[mT])
                        kb.barrier()
                        kb.replay(block)
                with ExitStack() as pc:
                    g1 = kb.sb([128, D], F32, pc)
                    sc2p = kb.sb([128, D], F32, pc)
                    sh2 = kb.sb([128, D], F32, pc)
                    l1g = kb.sb([128, D], F32, pc)
                    l1b = kb.sb([128, D], F32, pc)
                    XT = kb.sb([128, D], F32, pc)
                    PT = kb.sb([128, D], F32, pc)
                    pre = kb.sb([128, D], F32, pc)
                    x1 = kb.sb([128, D], F32, pc)
                    h2 = kb.sb([128, D], F32, pc)
                    h2Tf = kb.sb([128, 16, 128], F32, pc)
                    lg = kb.sb([128, 32], F32, pc)
                    m8 = kb.sb([128, 8], F32, pc)
                    nm = kb.sb([128, 1], F32, pc)
                    ex = kb.sb([128, 32], F32, pc)
                    msk = kb.sb([128, 32], F32, pc)
                    ssum = kb.sb([128, 1], F32, pc)
                    pW = [kb.ps([128, 512], F32, pc) for _ in range(4)]
                    pTf = [kb.ps([128, 4, 128], F32, pc) for _ in range(2)]
                    pR = kb.ps([128, 32], F32, pc)
                    modload(g1, g, 2)
                    modload(sc2p, g, 4, True)
                    modload(sh2, g, 3)
                    kb.dma("sp", l1g[:], T.lnp.ap()[0], writes=[l1g])
                    kb.dma("sp", l1b[:], T.lnp.ap()[1], writes=[l1b])
                    nload = [0]

                    def wloadO(i):
                        cc = i % 4
                        w = wch[i % 2]
                        kb.dma("pool", bview(w[:, :], 16), T.w_o.ap()[:, cc * 512:(cc + 1) * 512].rearrange("(kt p) c -> p kt c", p=128), writes=[w])
                    wloadO(0)
                    for t in range(4):
                        load_x(g, t, XT, PT)
                        for cc in range(4):
                            i = t * 4 + cc
                            if i + 1 < 16:
                                wloadO(i + 1)
                            w = wch[i % 2]
                            wv = bview(w[:, :], 16)
                            for kt in range(KT):
                                kb.op("pe", lambda e, kt=kt, t=t, cc=cc, wv=wv: e.matmul(out=pW[cc][:], lhsT=mT[:, kt, t * 128:(t + 1) * 128], rhs=wv[:, kt, :],
                                                                                       start=(kt == 0), stop=(kt == KT - 1)), reads=[w, mT], writes=[pW[cc]])
                            kb.op("dve", lambda e, cc=cc: e.tensor_tensor(out=pre[:, cc * 512:(cc + 1) * 512], in0=pW[cc][:], in1=g1[:, cc * 512:(cc + 1) * 512],
                                                                         op=ALU.mult), reads=[pW[cc], g1, pre], writes=[pre])
                        kb.op("dve", lambda e: e.scalar_tensor_tensor(out=pre[:], in0=XT[:], scalar=ALPHA, in1=pre[:], op0=ALU.mult, op1=ALU.add),
                              reads=[XT, pre], writes=[pre])
                        layernorm(pre, l1g, l1b, x1)
                        kb.dma("sp", T.x1D.ap()[(g * 4 + t) * 128:(g * 4 + t + 1) * 128, :], x1[:], reads=[x1])
                        kb.op("dve", lambda e: e.tensor_tensor(out=h2[:], in0=x1[:], in1=sc2p[:], op=ALU.mult), reads=[x1, sc2p], writes=[h2])
                        kb.op("pool", lambda e: e.tensor_tensor(out=h2[:], in0=h2[:], in1=sh2[:], op=ALU.add), reads=[h2, sh2], writes=[h2])
                        for q in range(4):
                            pt_ = pTf[q % 2]
                            for jj in range(4):
                                kt = q * 4 + jj
                                kb.op("pe", lambda e, kt=kt, jj=jj, pt_=pt_: e.transpose(out=pt_[:, jj, :], in_=h2[:, kt * 128:(kt + 1) * 128], identity=ident_f),
                                      reads=[h2, cst], writes=[pt_])
                            kb.op("act", lambda e, q=q, pt_=pt_: e.copy(out=h2Tf[:, q * 4:(q + 1) * 4, :], in_=pt_[:]), reads=[pt_, h2Tf], writes=[h2Tf])
                            kb.op("dve", lambda e, q=q, t=t, pt_=pt_: e.tensor_copy(out=hT[:, q * 4:(q + 1) * 4, t * 128:(t + 1) * 128], in_=pt_[:]),
                                  reads=[pt_, hT], writes=[hT])
                        for kt in range(KT):
                            kb.op("pe", lambda e, kt=kt: e.matmul(out=pR[:, 0:32], lhsT=h2Tf[:, kt, :], rhs=rw[:, kt, :], start=(kt == 0), stop=(kt == KT - 1)),
                                  reads=[h2Tf, rw], writes=[pR])
                        kb.op("dve", lambda e: e.tensor_tensor(out=lg[:], in0=pR[:, 0:32], in1=rbt[:], op=ALU.add), reads=[pR, rbt], writes=[lg])
                        kb.op("dve", lambda e: e.max(out=m8[:], in_=lg[:]), reads=[lg], writes=[m8])
                        kb.op("dve", lambda e: e.tensor_scalar(out=msk[:], in0=lg[:], scalar1=m8[:, 3:4], scalar2=None, op0=ALU.is_ge),
                              reads=[lg, m8], writes=[msk])
                        kb.op("dve", lambda e: e.tensor_scalar(out=nm[:], in0=m8[:, 0:1], scalar1=-1.0, scalar2=None, op0=ALU.mult), reads=[m8], writes=[nm])
                        kb.op("act", lambda e: e.activation(out=ex[:], in_=lg[:], func=AF.Exp, bias=nm[:, 0:1], scale=1.0), reads=[lg, nm], writes=[ex])
                        kb.op("dve", lambda e: e.tensor_tensor(out=ex[:], in0=ex[:], in1=msk[:], op=ALU.mult), reads=[ex, msk], writes=[ex])
                        kb.op("dve", lambda e: e.reduce_sum(out=ssum[:], in_=ex[:], axis=AX.X), reads=[ex], writes=[ssum])
                        kb.op("dve", lambda e: e.reciprocal(out=ssum[:], in_=ssum[:]), reads=[ssum], writes=[ssum])
                        kb.op("dve", lambda e, t=t: e.tensor_scalar(out=Gall[:, g * 4 + t, :], in0=ex[:], scalar1=ssum[:, 0:1], scalar2=None, op0=ALU.mult),
                              reads=[ex, ssum, Gall], writes=[Gall])
                    kb.barrier()
                    kb.replay(block)
                with ExitStack() as pm:
                    yacc = kb.sb([128, 4, D], F32, pm)
                    actT = kb.sb([128, 16, 512], BF16, pm)
                    GT = kb.sb([32, 4, 128], F32, pm)
                    tg = [kb.sb([128, 512], F32, pm) for _ in range(2)]
                    tsg = [kb.sb([128, 512], F32, pm) for _ in range(2)]
                    tu = [kb.sb([128, 512], F32, pm) for _ in range(2)]
                    pG = [kb.ps([128, 512], F32, pm) for _ in range(4)]
                    pD = [kb.ps([128, 512], F32, pm) for _ in range(2)]
                    pX = kb.ps([128, 512], F32, pm)
                    for t in range(4):
                        kb.op("pe", lambda e, t=t: e.transpose(out=pX[0:32, 0:128], in_=Gall[:, g * 4 + t, :], identity=ident_f), reads=[Gall, cst], writes=[pX])
                        kb.op("act", lambda e, t=t: e.copy(out=GT[:, t, :], in_=pX[0:32, 0:128]), reads=[pX, GT], writes=[GT])
                    for t in range(4):
                        for cc in range(4):
                            pd = pD[(t * 4 + cc) % 2]
                            kb.op("pe", lambda e, t=t, cc=cc, pd=pd: e.matmul(out=pd[:], lhsT=GT[:, t, :], rhs=bdn[:, cc * 512:(cc + 1) * 512], start=True, stop=True),
                                  reads=[GT, bdn], writes=[pd])
                            kb.op("act", lambda e, t=t, cc=cc, pd=pd: e.copy(out=yacc[:, t, cc * 512:(cc + 1) * 512], in_=pd[:]), reads=[pd, yacc], writes=[yacc])
                    chunks = []
                    for ex_ in range(nexp):
                        for fc in range(8):
                            chunks.append((ex_, "gu", fc))
                        for cc in range(4):
                            chunks.append((ex_, "dn", cc))

                    def wloadM(i):
                        ex_, kind, c = chunks[i]
                        w = wch[i % 2]
                        r_, q_ = ex_ // 4, ex_ % 4
                        if kind == "gu":
                            srcg = T.wgu_G[q_][0].ap()[r_ * D:(r_ + 1) * D, :]
                            srcu = T.wgu_G[q_][1].ap()[r_ * D:(r_ + 1) * D, :]
                            kb.dma("pool", bview(w[:, 0:4096], 16), srcg[:, c * 256:(c + 1) * 256].rearrange("(kt p) c -> p kt c", p=128), writes=[w])
                            kb.dma("pool", bview(w[:, 4096:8192], 16), srcu[:, c * 256:(c + 1) * 256].rearrange("(kt p) c -> p kt c", p=128), writes=[w])
                        else:
                            src = T.wdn_G[q_].ap()[r_ * D:(r_ + 1) * D, :]
                            kb.dma("pool", bview(w[:, :], 16), src[:, c * 512:(c + 1) * 512].rearrange("(kt p) c -> p kt c", p=128), writes=[w])
                    wloadM(0)
                    for i, (ex_, kind, c) in enumerate(chunks):
                        if i + 1 < len(chunks):
                            wloadM(i + 1)
                        w = wch[i % 2]
                        if kind == "gu":
                            wg = bview(w[:, 0:4096], 16)
                            wu = bview(w[:, 4096:8192], 16)
                            for j in range(2):
                                f = c * 2 + j
                                pg_, pu_ = pG[j], pG[2 + j]
                                for kt in range(KT):
                                    kb.op("pe", lambda e, kt=kt, j=j, wg=wg, pg_=pg_: e.matmul(out=pg_[:], lhsT=wg[:, kt, j * 128:(j + 1) * 128], rhs=hT[:, kt, :],
                                                                                             start=(kt == 0), stop=(kt == KT - 1)), reads=[w, hT], writes=[pg_])
                                for kt in range(KT):
                                    kb.op("pe", lambda e, kt=kt, j=j, wu=wu, pu_=pu_: e.matmul(out=pu_[:], lhsT=wu[:, kt, j * 128:(j + 1) * 128], rhs=hT[:, kt, :],
                                                                                             start=(kt == 0), stop=(kt == KT - 1)), reads=[w, hT], writes=[pu_])
                                bgc = ex_ * 32 + f
                                buc = ex_ * 32 + 16 + f
                                a_, s_, u_ = tg[j], tsg[j], tu[j]
                                kb.op("act", lambda e, pg_=pg_, a_=a_, bgc=bgc: e.activation(out=a_[:], in_=pg_[:], func=AF.Identity, bias=bgu_t[:, bgc:bgc + 1], scale=1.0),
                                      reads=[pg_, bgu_t], writes=[a_])
                                kb.op("dve", lambda e, a_=a_: e.tensor_scalar(out=a_[:], in0=a_[:], scalar1=7.0, scalar2=None, op0=ALU.min), reads=[a_], writes=[a_])
                                kb.op("act", lambda e, a_=a_, s_=s_: e.activation(out=s_[:], in_=a_[:], func=AF.Sigmoid, scale=1.702), reads=[a_], writes=[s_])
                                kb.op("act", lambda e, pu_=pu_, u_=u_, buc=buc: e.activation(out=u_[:], in_=pu_[:], func=AF.Identity, bias=bgu_t[:, buc:buc + 1], scale=1.0),
                                      reads=[pu_, bgu_t], writes=[u_])
                                kb.op("dve", lambda e, u_=u_: e.tensor_scalar(out=u_[:], in0=u_[:], scalar1=7.0, scalar2=-7.0, op0=ALU.min, op1=ALU.max), reads=[u_], writes=[u_])
                                kb.op("dve", lambda e, u_=u_, a_=a_: e.scalar_tensor_tensor(out=u_[:], in0=u_[:], scalar=1.0, in1=a_[:], op0=ALU.add, op1=ALU.mult),
                                      reads=[u_, a_], writes=[u_])
                                kb.op("pool", lambda e, u_=u_, s_=s_, f=f: e.tensor_tensor(out=actT[:, f, :], in0=u_[:], in1=s_[:], op=ALU.mult),
                                      reads=[u_, s_, actT], writes=[actT])
                        else:
                            wd = bview(w[:, :], 16)
                            for t in range(4):
                                pd = pD[(c * 4 + t) % 2]
                                for kt in range(KT):
                                    kb.op("pe", lambda e, kt=kt, t=t, wd=wd, pd=pd: e.matmul(out=pd[:], lhsT=actT[:, kt, t * 128:(t + 1) * 128], rhs=wd[:, kt, :],
                                                                                           start=(kt == 0), stop=(kt == KT - 1)), reads=[w, actT], writes=[pd])
                                kb.op("dve", lambda e, t=t, c=c, pd=pd, ex_=ex_: e.scalar_tensor_tensor(
                                    out=yacc[:, t, c * 512:(c + 1) * 512], in0=pd[:], scalar=Gall[:, g * 4 + t, ex_:ex_ + 1],
                                    in1=yacc[:, t, c * 512:(c + 1) * 512], op0=ALU.mult, op1=ALU.add), reads=[pd, Gall, yacc], writes=[yacc])
                    with ExitStack() as pf:
                        g2t = kb.sb([128, D], F32, pf)
                        l2g = kb.sb([128, D], F32, pf)
                        l2b = kb.sb([128, D], F32, pf)
                        x1t = kb.sb([128, D], F32, pf)
                        pre2 = kb.sb([128, D], F32, pf)
                        outt = kb.sb([128, D], F32, pf)
                        modload(g2t, g, 5)
                        kb.dma("sp", l2g[:], T.lnp.ap()[2], writes=[l2g])
                        kb.dma("sp", l2b[:], T.lnp.ap()[3], writes=[l2b])
                        for t in range(4):
                            r0 = (g * 4 + t) * 128
                            kb.dma("sp", x1t[:], T.x1D.ap()[r0:r0 + 128, :], writes=[x1t])
                            kb.op("dve", lambda e, t=t: e.tensor_tensor(out=pre2[:], in0=yacc[:, t, :], in1=g2t[:], op=ALU.mult), reads=[yacc, g2t], writes=[pre2])
                            kb.op("dve", lambda e: e.scalar_tensor_tensor(out=pre2[:], in0=x1t[:], scalar=ALPHA, in1=pre2[:], op0=ALU.mult, op1=ALU.add),
                                  reads=[x1t, pre2], writes=[pre2])
                            layernorm(pre2, l2g, l2b, outt)
                            kb.dma("sp", T.y_out.ap()[r0:r0 + 128, :], outt[:], reads=[outt])
                        kb.barrier()
                        kb.replay(block)
                    kb.barrier()
                    kb.replay(block)
                kb.barrier()
                kb.replay(block)
        kb.barrier()
        kb.replay(block)


_CACHE = {}


def kernel(**inputs):
    inp = {k: np.asarray(v) for k, v in inputs.items()}
    maps = prep(inp, "all")
    if "nc" not in _CACHE:
        _CACHE["nc"] = build("all")
    nc, dbg = _CACHE["nc"]
    names = dbg["_inputs"]
    in_maps = [{k: np.ascontiguousarray(m[k]) for k in names} for m in maps]
    res = run_bass_kernel_spmd(nc, in_maps, core_ids=list(range(NCORES)))
    y = np.zeros((NTOK, D), np.float32)
    nsf = np.zeros((16, 1, 32, 128, 128), np.float32)
    nsb = np.zeros((16, 1, 32, 128, 128), np.float32)
    for c in range(NCORES):
        r = res.results[c]
        yo = np.asarray(r["y_out"], np.float32)
        for part in range(3):
            y[part * 4096 + 512 * c: part * 4096 + 512 * c + 512] = yo[part * 512:(part + 1) * 512]
        ns = np.asarray(r["ns_out"], np.float32)
        nsf[:, 0, 4 * c:4 * c + 4] = ns[0]
        nsb[:, 0, 4 * c:4 * c + 4] = ns[1]
    y_prompt = np.ascontiguousarray(y[:4096].reshape(16, 256, D))
    y_sample = np.ascontiguousarray(y[4096:].reshape(2, 4096, D))
    return (y_prompt, y_sample, nsf, nsb)
```
